# Optimizing a Trainium2 kernel written in Bass

```python
import math
import jax
import jax.numpy as jnp
from jax import lax
import numpy as np

D_MODEL = 1024
BATCH = 4
SEQ = 8192
DEPTH = 2

GRID_W = 64
CTX_LEN = 256
NORM_EPS = 1e-6

RWKV_HEADS = 8
RWKV_HEAD_DIM = 64
RWKV_WIDTH = RWKV_HEADS * RWKV_HEAD_DIM
W_LORA = 64
A_LORA = 64
G_LORA = 128
RWKV_IN = 3 * RWKV_WIDTH + W_LORA + A_LORA + G_LORA
DECAY_SCALE = math.exp(-0.5)
GN_EPS = 64e-5
SHORT_CONV = 3

S5_GROUP = 16
S5_GROUPS = 32
S5_WIDTH = S5_GROUP * S5_GROUPS
S5_STATE = 64
DT_MIN = 1e-3
DT_MAX = 1e-1

HYB_IN = RWKV_IN + S5_WIDTH
HYB_OUT = RWKV_WIDTH + S5_WIDTH

MLA_HEADS = 16
Q_LORA = 256
KV_LORA = 128
QK_NOPE = 64
QK_ROPE = 32
V_DIM = 64
MLA_IN = Q_LORA + KV_LORA + QK_ROPE
MLA_OUT = MLA_HEADS * V_DIM
MLA_SCALE = (QK_NOPE + QK_ROPE) ** -0.5
ROPE_AXIS_DIMS = QK_ROPE // 2
ROPE_BASE = 10000.0
Q_BLOCK = 128

N_EXPERTS = 16
N_GROUPS = 4
EXPERTS_PER_GROUP = N_EXPERTS // N_GROUPS
TOP_K = 2
D_EXPERT = 256

kernel_name = 'hybrid_rwkv7_s5_mla_moe_diffusion_block'


def rmsnorm(x, g):
    xf = x.astype(jnp.float32)
    y = xf * lax.rsqrt(jnp.mean(jnp.square(xf), -1, keepdims=True) + NORM_EPS)
    return (y * g.astype(jnp.float32)).astype(x.dtype)


def modulate(h, shift, scale):
    return h * (1.0 + scale) + shift


def swiglu(t, wg, wu, wd):
    return (jax.nn.silu(t @ wg) * (t @ wu)) @ wd


def centred_depthwise_conv(x, w):
    return lax.conv_general_dilated(x, w[:, None, :], window_strides=(1,), padding='SAME',
                                    dimension_numbers=('NWC', 'WIO', 'NWC'),
                                    feature_group_count=x.shape[-1])


def rwkv_inputs(p, conv_w, w0, w_up, a0, a_up, g_up, k_k, k_a):
    p = centred_depthwise_conv(p, conv_w).astype(jnp.float32)
    r, k, v, wd, ad, gd = jnp.split(p, [RWKV_WIDTH, 2 * RWKV_WIDTH, 3 * RWKV_WIDTH,
                                        3 * RWKV_WIDTH + W_LORA, 3 * RWKV_WIDTH + W_LORA + A_LORA], axis=-1)
    heads = lambda t: t.reshape(t.shape[:-1] + (RWKV_HEADS, RWKV_HEAD_DIM))
    w = jnp.exp(-DECAY_SCALE * jax.nn.sigmoid(w0[:, None, None, :] + jnp.einsum('blr,zrc->zblc', jnp.tanh(wd), w_up)))
    a = jax.nn.sigmoid(a0[:, None, None, :] + jnp.einsum('blr,zrc->zblc', ad, a_up))
    g = jax.nn.sigmoid(gd) @ g_up
    kk = heads(k * k_k)
    kk = kk * lax.rsqrt(jnp.sum(jnp.square(kk), -1, keepdims=True) + 1e-12)
    k_dir = k * (1.0 + (a - 1.0) * k_a)
    return heads(r), heads(w), heads(k_dir), heads(v), kk, heads(a), g, heads(k)


def _rwkv_step(S, inp):
    r, w, k, v, kk, a = inp
    sa = jnp.einsum('zbhvk,zbhk->zbhv', S, kk)
    S = S * w[..., None, :] - sa[..., None] * (a * kk)[..., None, :] + v[..., None] * k[..., None, :]
    return S, jnp.einsum('zbhvk,zbhk->zbhv', S, r)


def rwkv_bidir(r, w, k, v, kk, a, s0):
    both = lambda t: jnp.stack([t, jnp.flip(t, 1)])
    per_dir = lambda t: jnp.stack([t[0], jnp.flip(t[1], 1)])
    xs = (both(r), per_dir(w), per_dir(k), both(v), both(kk), per_dir(a))
    xs = tuple(jnp.moveaxis(t, 2, 0) for t in xs)
    s_fin, ys = lax.scan(_rwkv_step, s0, xs)
    y = ys[:, 0] + jnp.flip(ys[:, 1], 0)
    return jnp.moveaxis(y, 0, 1), s_fin


def rwkv_output(y, rin, r_k, ln_w, ln_b):
    r, _, _, v, _, _, g, k = rin
    mu = jnp.mean(y, -1, keepdims=True)
    var = jnp.mean(jnp.square(y - mu), -1, keepdims=True)
    yn = ((y - mu) * lax.rsqrt(var + GN_EPS)).reshape(y.shape[:2] + (RWKV_WIDTH,)) * ln_w + ln_b
    bonus = (jnp.sum(r * k * r_k, -1, keepdims=True) * v).reshape(yn.shape)
    return (yn + bonus) * g


def s5_discretise(lam_re, lam_im, log_dt, b_re, b_im, c_re, c_im):
    f32 = jnp.float32
    lam = lax.complex(lam_re.astype(f32), lam_im.astype(f32))
    dt = jnp.exp(log_dt.astype(f32))[..., None]
    lam_bar = jnp.exp(lam * dt)
    b = lax.complex(b_re.astype(f32), b_im.astype(f32))
    b_bar = ((lam_bar - 1.0) / lam)[..., None] * b
    cmat = lax.complex(c_re.astype(f32), c_im.astype(f32))
    return lam_bar, b_bar, cmat


def _linear_combine(e1, e2):
    a1, b1 = e1
    a2, b2 = e2
    return a1 * a2, a2 * b1 + b2


def s5_scan(u, lam_bar, b_bar, h0):
    bu = jnp.einsum('lbgc,gpc->lbgp', u.astype(jnp.complex64), b_bar)
    a = jnp.broadcast_to(lam_bar, (u.shape[0], 1) + lam_bar.shape)
    cum_a, h = lax.associative_scan(_linear_combine, (a, bu), axis=0)
    return h + cum_a * h0[None]


def s5_bidir(u, lam_bar, b_bar, cmat, d, h0_f, h0_b):
    bsz, n = u.shape[:2]
    ut = jnp.transpose(u.astype(jnp.float32).reshape(bsz, n, S5_GROUPS, S5_GROUP), (1, 0, 2, 3))
    hf = s5_scan(ut, lam_bar[0], b_bar[0], h0_f)
    hb = s5_scan(jnp.flip(ut, 0), lam_bar[1], b_bar[1], h0_b)
    y = (jnp.real(jnp.einsum('lbgp,gcp->lbgc', hf, cmat[0]))
         + jnp.flip(jnp.real(jnp.einsum('lbgp,gcp->lbgc', hb, cmat[1])), 0)
         + d.astype(jnp.float32).reshape(S5_GROUPS, S5_GROUP) * ut)
    return jnp.transpose(y, (1, 0, 2, 3)), hf[-1], hb[-1]


def s5_glu(y, glu_w, glu_b):
    z = jax.nn.gelu(y)
    gate = jax.nn.sigmoid(jnp.einsum('blgc,gce->blge', z, glu_w) + glu_b.reshape(S5_GROUPS, S5_GROUP))
    return (z * gate).reshape(y.shape[:2] + (S5_WIDTH,))


def rwkv_s5_mixer(hc, hl, w_in, w_out, conv_w, w0, w_up, a0, a_up, g_up, k_k, k_a, r_k, ln_w, ln_b,
                  lam_re, lam_im, log_dt, b_re, b_im, c_re, c_im, d, glu_w, glu_b, with_ctx_out):
    bsz = hl.shape[0]
    pc = hc @ w_in
    pl = hl @ w_in
    rc = rwkv_inputs(pc[..., :RWKV_IN], conv_w, w0, w_up, a0, a_up, g_up, k_k, k_a)
    rl = rwkv_inputs(pl[..., :RWKV_IN], conv_w, w0, w_up, a0, a_up, g_up, k_k, k_a)
    s0 = jnp.zeros((2, bsz, RWKV_HEADS, RWKV_HEAD_DIM, RWKV_HEAD_DIM), jnp.float32)
    yc, s_ctx = rwkv_bidir(*rc[:6], s0)
    yl, _ = rwkv_bidir(*rl[:6], s_ctx)
    lam_bar, b_bar, cmat = s5_discretise(lam_re, lam_im, log_dt, b_re, b_im, c_re, c_im)
    h0 = jnp.zeros((bsz, S5_GROUPS, S5_STATE), jnp.complex64)
    zc, hf_c, hb_c = s5_bidir(pc[..., RWKV_IN:], lam_bar, b_bar, cmat, d, h0, h0)
    zl, _, _ = s5_bidir(pl[..., RWKV_IN:], lam_bar, b_bar, cmat, d, hf_c, hb_c)

    def merge(y, rin, z, dtype):
        cat = jnp.concatenate([rwkv_output(y, rin, r_k, ln_w, ln_b), s5_glu(z, glu_w, glu_b)], -1)
        return cat.astype(dtype) @ w_out

    o_l = merge(yl, rl, zl, hl.dtype)
    o_c = merge(yc, rc, zc, hc.dtype) if with_ctx_out else None
    return o_c, o_l


def axial_rope_tables(n_tokens):
    rows = n_tokens // GRID_W
    row = jnp.repeat(jnp.arange(rows, dtype=jnp.float32), GRID_W)
    col = jnp.tile(jnp.arange(GRID_W, dtype=jnp.float32), rows)
    inv_freq = ROPE_BASE ** (-jnp.arange(0, ROPE_AXIS_DIMS, 2, dtype=jnp.float32) / ROPE_AXIS_DIMS)
    ang = jnp.concatenate([row[:, None] * inv_freq, col[:, None] * inv_freq], -1)
    return jnp.cos(ang), jnp.sin(ang)


def apply_axial_rope(x, cos, sin):
    half = QK_ROPE // 2
    x1 = x[..., :half].astype(jnp.float32)
    x2 = x[..., half:].astype(jnp.float32)
    c, s = cos[:, None, :], sin[:, None, :]
    return jnp.concatenate([x1 * c - x2 * s, x1 * s + x2 * c], -1).astype(x.dtype)


def mla_q(q_down, q_norm, q_up, rope):
    q = (rmsnorm(q_down, q_norm) @ q_up).reshape(q_down.shape[:2] + (MLA_HEADS, QK_NOPE + QK_ROPE))
    q_nope, q_pe = q[..., :QK_NOPE], q[..., QK_NOPE:]
    if rope is not None:
        q_pe = apply_axial_rope(q_pe, *rope)
    return jnp.concatenate([q_nope, q_pe], -1)


def mla_kv(kv_down, k_pe, kv_norm, kv_up, rope):
    kv = (rmsnorm(kv_down, kv_norm) @ kv_up).reshape(kv_down.shape[:2] + (MLA_HEADS, QK_NOPE + V_DIM))
    k_nope, v = kv[..., :QK_NOPE], kv[..., QK_NOPE:]
    k_pe = k_pe[:, :, None, :]
    if rope is not None:
        k_pe = apply_axial_rope(k_pe, *rope)
    k = jnp.concatenate([k_nope, jnp.broadcast_to(k_pe, k_nope.shape[:-1] + (QK_ROPE,))], -1)
    return k, v


def latent_attention(q, k_lat, v_lat, k_ctx, v_ctx):
    k_all = jnp.concatenate([k_ctx, k_lat], 1)
    v_all = jnp.concatenate([v_ctx, v_lat], 1)
    bsz, n, h, dq = q.shape
    qb = jnp.moveaxis(q.reshape(bsz, n // Q_BLOCK, Q_BLOCK, h, dq), 1, 0)

    def block(qblk):
        s = jnp.einsum('bqhd,bkhd->bhqk', qblk, k_all, preferred_element_type=jnp.float32) * MLA_SCALE
        p = jax.nn.softmax(s, axis=-1).astype(v_all.dtype)
        return jnp.einsum('bhqk,bkhd->bqhd', p, v_all)

    o = lax.map(block, qb)
    return jnp.moveaxis(o, 0, 1).reshape(bsz, n, h * V_DIM)


def context_attention(q, k, v):
    s = jnp.einsum('bqhd,bkhd->bhqk', q, k, preferred_element_type=jnp.float32) * MLA_SCALE
    p = jax.nn.softmax(s, axis=-1).astype(v.dtype)
    o = jnp.einsum('bhqk,bkhd->bqhd', p, v)
    return o.reshape(q.shape[:2] + (MLA_HEADS * V_DIM,))


def mla_mixer(hc, hl, w_in, q_norm, q_up, kv_norm, kv_up, w_out, with_ctx_out):
    rope = axial_rope_tables(hl.shape[1])
    pl = hl @ w_in
    ql = mla_q(pl[..., :Q_LORA], q_norm, q_up, rope)
    kl, vl = mla_kv(pl[..., Q_LORA:Q_LORA + KV_LORA], pl[..., Q_LORA + KV_LORA:], kv_norm, kv_up, rope)
    pc = hc @ w_in[:, Q_LORA:]
    kc, vc = mla_kv(pc[..., :KV_LORA], pc[..., KV_LORA:], kv_norm, kv_up, None)
    o_l = latent_attention(ql, kl, vl, kc, vc) @ w_out
    o_c = None
    if with_ctx_out:
        qc = mla_q(hc @ w_in[:, :Q_LORA], q_norm, q_up, None)
        o_c = context_attention(qc, kc, vc) @ w_out
    return o_c, o_l


def moe_ffn(h, router_w, router_b, ex_gate, ex_up, ex_down, sh_gate, sh_up, sh_down):
    shp = h.shape
    t = h.reshape(-1, shp[-1])
    scores = jax.nn.sigmoid(jnp.einsum('td,de->te', t, router_w, preferred_element_type=jnp.float32))
    biased = (scores + router_b.astype(jnp.float32)).reshape(-1, N_GROUPS, EXPERTS_PER_GROUP)
    group_score = jnp.sum(lax.top_k(biased, 2)[0], -1)
    g_sel = jnp.argmax(group_score, -1)
    in_group = jnp.take_along_axis(biased, g_sel[:, None, None], axis=1)[:, 0]
    _, local = lax.top_k(in_group, TOP_K)
    idx = g_sel[:, None] * EXPERTS_PER_GROUP + local
    w_sel = jnp.take_along_axis(scores, idx, axis=1)
    w_sel = w_sel / jnp.sum(w_sel, -1, keepdims=True)
    combine = jnp.einsum('tke,tk->te', jax.nn.one_hot(idx, N_EXPERTS, dtype=jnp.float32), w_sel).astype(t.dtype)
    out = swiglu(t, sh_gate, sh_up, sh_down)
    for e in range(N_EXPERTS):
        out = out + combine[:, e:e + 1] * swiglu(t, ex_gate[e], ex_up[e], ex_down[e])
    return out.reshape(shp)


def setup_inputs(seed: int = 0) -> dict:
    key = jax.random.key(seed)
    keys = iter(jax.random.split(key, 64))
    f32 = jnp.float32

    def nrm(shape, scale):
        return scale * jax.random.normal(next(keys), shape, f32)

    D = D_MODEL
    NE = (DEPTH + 1) // 2
    NO = DEPTH // 2
    G, C, P = S5_GROUPS, S5_GROUP, S5_STATE
    E, F = N_EXPERTS, D_EXPERT
    return {
        'x': nrm((BATCH, SEQ, D), 1.0),
        'c': nrm((BATCH, D), 1.0),
        'ctx': nrm((BATCH, CTX_LEN, D), 1.0),
        'c_ctx': nrm((D,), 1.0),
        'mod_w': nrm((DEPTH, D, 6 * D), 0.5 * D ** -0.5),
        'mod_b': nrm((DEPTH, 6 * D), 0.02),
        'norm1_g': 1.0 + nrm((DEPTH, D), 0.02),
        'norm2_g': 1.0 + nrm((DEPTH, D), 0.02),
        'final_g': 1.0 + nrm((D,), 0.02),
        'hy_w_in': nrm((NE, D, HYB_IN), D ** -0.5),
        'hy_w_out': nrm((NE, HYB_OUT, D), HYB_OUT ** -0.5),
        'rk_conv': jnp.array([0.25, 0.5, 0.25], f32)[None, :, None] + nrm((NE, SHORT_CONV, RWKV_IN), 0.05),
        'rk_w0': nrm((NE, 2, RWKV_WIDTH), 1.0) - 0.5,
        'rk_w_up': nrm((NE, 2, W_LORA, RWKV_WIDTH), W_LORA ** -0.5),
        'rk_a0': nrm((NE, 2, RWKV_WIDTH), 0.5),
        'rk_a_up': nrm((NE, 2, A_LORA, RWKV_WIDTH), 0.5 * A_LORA ** -0.5),
        'rk_g_up': nrm((NE, G_LORA, RWKV_WIDTH), G_LORA ** -0.5),
        'rk_k_k': 0.85 + nrm((NE, RWKV_WIDTH), 0.05),
        'rk_k_a': 1.0 + nrm((NE, RWKV_WIDTH), 0.05),
        'rk_r_k': nrm((NE, RWKV_HEADS, RWKV_HEAD_DIM), 0.1),
        'rk_ln_w': 1.0 + nrm((NE, RWKV_WIDTH), 0.02),
        'rk_ln_b': nrm((NE, RWKV_WIDTH), 0.02),
        's5_lam_re': -0.5 + nrm((NE, 2, G, P), 0.02),
        's5_lam_im': jnp.broadcast_to(jnp.pi * jnp.arange(P, dtype=f32), (NE, 2, G, P)),
        's5_log_dt': jax.random.uniform(next(keys), (NE, 2, G), f32, math.log(DT_MIN), math.log(DT_MAX)),
        's5_b_re': nrm((NE, 2, G, P, C), (2 * C) ** -0.5),
        's5_b_im': nrm((NE, 2, G, P, C), (2 * C) ** -0.5),
        's5_c_re': nrm((NE, 2, G, C, P), P ** -0.5),
        's5_c_im': nrm((NE, 2, G, C, P), P ** -0.5),
        's5_d': nrm((NE, S5_WIDTH), 1.0),
        's5_glu_w': nrm((NE, G, C, C), C ** -0.5),
        's5_glu_b': nrm((NE, S5_WIDTH), 0.02),
        'mla_w_in': nrm((NO, D, MLA_IN), D ** -0.5),
        'mla_q_norm': 1.0 + nrm((NO, Q_LORA), 0.02),
        'mla_q_up': nrm((NO, Q_LORA, MLA_HEADS * (QK_NOPE + QK_ROPE)), Q_LORA ** -0.5),
        'mla_kv_norm': 1.0 + nrm((NO, KV_LORA), 0.02),
        'mla_kv_up': nrm((NO, KV_LORA, MLA_HEADS * (QK_NOPE + V_DIM)), KV_LORA ** -0.5),
        'mla_w_out': nrm((NO, MLA_OUT, D), MLA_OUT ** -0.5),
        'router_w': nrm((D, E), D ** -0.5),
        'router_b': nrm((E,), 0.01),
        'ex_gate': nrm((DEPTH, E, D, F), D ** -0.5),
        'ex_up': nrm((DEPTH, E, D, F), D ** -0.5),
        'ex_down': nrm((DEPTH, E, F, D), F ** -0.5),
        'sh_gate': nrm((DEPTH, D, F), D ** -0.5),
        'sh_up': nrm((DEPTH, D, F), D ** -0.5),
        'sh_down': nrm((DEPTH, F, D), F ** -0.5),
    }


def reference(x, c, ctx, c_ctx, mod_w, mod_b, norm1_g, norm2_g, final_g,
              hy_w_in, hy_w_out, rk_conv, rk_w0, rk_w_up, rk_a0, rk_a_up, rk_g_up, rk_k_k, rk_k_a, rk_r_k,
              rk_ln_w, rk_ln_b, s5_lam_re, s5_lam_im, s5_log_dt, s5_b_re, s5_b_im, s5_c_re, s5_c_im, s5_d,
              s5_glu_w, s5_glu_b, mla_w_in, mla_q_norm, mla_q_up, mla_kv_norm, mla_kv_up, mla_w_out,
              router_w, router_b, ex_gate, ex_up, ex_down, sh_gate, sh_up, sh_down):
    h_lat, h_ctx = x, ctx
    silu_c = jax.nn.silu(c)
    silu_cc = jax.nn.silu(c_ctx)
    for layer in range(DEPTH):
        last = layer == DEPTH - 1
        i = layer // 2
        m_l = jnp.split((silu_c @ mod_w[layer] + mod_b[layer])[:, None, :], 6, axis=-1)
        m_c = jnp.split(silu_cc @ mod_w[layer] + mod_b[layer], 6, axis=-1)
        a_l = modulate(rmsnorm(h_lat, norm1_g[layer]), m_l[0], m_l[1])
        a_c = modulate(rmsnorm(h_ctx, norm1_g[layer]), m_c[0], m_c[1])
        if layer % 2 == 0:
            o_c, o_l = rwkv_s5_mixer(a_c, a_l, hy_w_in[i], hy_w_out[i], rk_conv[i], rk_w0[i], rk_w_up[i],
                                     rk_a0[i], rk_a_up[i], rk_g_up[i], rk_k_k[i], rk_k_a[i], rk_r_k[i],
                                     rk_ln_w[i], rk_ln_b[i], s5_lam_re[i], s5_lam_im[i], s5_log_dt[i],
                                     s5_b_re[i], s5_b_im[i], s5_c_re[i], s5_c_im[i], s5_d[i],
                                     s5_glu_w[i], s5_glu_b[i], not last)
        else:
            o_c, o_l = mla_mixer(a_c, a_l, mla_w_in[i], mla_q_norm[i], mla_q_up[i], mla_kv_norm[i],
                                 mla_kv_up[i], mla_w_out[i], not last)
        h_lat = h_lat + m_l[2] * o_l
        f_l = modulate(rmsnorm(h_lat, norm2_g[layer]), m_l[3], m_l[4])
        if last:
            h_lat = h_lat + m_l[5] * moe_ffn(f_l, router_w, router_b, ex_gate[layer], ex_up[layer],
                                             ex_down[layer], sh_gate[layer], sh_up[layer], sh_down[layer])
        else:
            h_ctx = h_ctx + m_c[2] * o_c
            f_c = modulate(rmsnorm(h_ctx, norm2_g[layer]), m_c[3], m_c[4])
            n_lat = f_l.shape[1]
            f = moe_ffn(jnp.concatenate([f_l, f_c], 1), router_w, router_b, ex_gate[layer], ex_up[layer],
                        ex_down[layer], sh_gate[layer], sh_up[layer], sh_down[layer])
            h_lat = h_lat + m_l[5] * f[:, :n_lat]
            h_ctx = h_ctx + m_c[5] * f[:, n_lat:]
    return rmsnorm(h_lat, final_g)
```

```python
import contextlib
import math
import numpy as np
import concourse.bass as bass
import concourse.mybir as mybir
from concourse.bass_utils import run_bass_kernel_spmd

F32 = mybir.dt.float32
BF16 = mybir.dt.bfloat16
ALU = mybir.AluOpType
AF = mybir.ActivationFunctionType
AX = mybir.AxisListType

NDMA_SEMS = 24
SAME_ENGINE_SYNC = True


class Prog:
    def __init__(self, nc):
        self.nc = nc
        self.ops = []
        self.stack = contextlib.ExitStack()
        self._n = 0

    def sb(self, shape, dt=F32, name=None):
        self._n += 1
        name = "s_" + (name or f"sb{self._n}")
        return self.stack.enter_context(self.nc.sbuf_tensor(name, list(shape), dt))

    def ps(self, shape, dt=F32, name=None):
        self._n += 1
        name = "p_" + (name or f"ps{self._n}")
        return self.stack.enter_context(self.nc.psum_tensor(name, list(shape), dt))

    @staticmethod
    def _key(a):
        if isinstance(a, (str, tuple)):
            return a
        t = getattr(a, "tensor", a)
        return t.name

    def op(self, eng, fn, r=(), w=(), dma=False):
        self.ops.append(dict(eng=eng, fn=fn, r=[self._key(x) for x in r],
                             w=[self._key(x) for x in w], dma=dma))

    def mm(self, out, lhsT, rhs, start=True, stop=True, r=None, w=None):
        self.op("pe", lambda e: e.matmul(out, lhsT, rhs, start=start, stop=stop),
                r=r if r is not None else [lhsT, rhs], w=w if w is not None else [out])

    def tr(self, out, in_, ident, r=None, w=None):
        self.op("pe", lambda e: e.transpose(out, in_, ident),
                r=r if r is not None else [in_, ident], w=w if w is not None else [out])

    def act(self, out, in_, func, bias=None, scale=None, accum_out=None, eng="act", r=None, w=None):
        kw = {}
        rr = [in_]
        if bias is not None:
            kw["bias"] = bias
            if not isinstance(bias, (int, float)):
                rr.append(bias)
        if scale is not None:
            kw["scale"] = scale
            if not isinstance(scale, (int, float)):
                rr.append(scale)
        ww = [out]
        if accum_out is not None:
            kw["accum_out"] = accum_out
            ww.append(accum_out)
        self.op(eng, lambda e: e.activation(out, in_, func, **kw),
                r=r if r is not None else rr, w=w if w is not None else ww)

    def ts(self, out, in0, s1, s2, op0, op1=None, accum_out=None, eng="dve", r=None, w=None):
        rr = [in0] + [s for s in (s1, s2) if s is not None and not isinstance(s, (int, float))]
        ww = [out] + ([accum_out] if accum_out is not None else [])
        kw = {}
        if accum_out is not None:
            kw["accum_out"] = accum_out
        if op1 is None:
            self.op(eng, lambda e: e.tensor_scalar(out, in0, s1, None, op0, **kw),
                    r=r if r is not None else rr, w=w if w is not None else ww)
        else:
            self.op(eng, lambda e: e.tensor_scalar(out, in0, s1, s2, op0, op1, **kw),
                    r=r if r is not None else rr, w=w if w is not None else ww)

    def tt(self, out, in0, in1, op, eng="dve", r=None, w=None):
        self.op(eng, lambda e: e.tensor_tensor(out, in0, in1, op),
                r=r if r is not None else [in0, in1], w=w if w is not None else [out])

    def stt(self, out, in0, scalar, in1, op0, op1, eng="dve", r=None, w=None):
        rr = [in0, in1] + ([scalar] if not isinstance(scalar, (int, float)) else [])
        self.op(eng, lambda e: e.scalar_tensor_tensor(out, in0, scalar, in1, op0, op1),
                r=r if r is not None else rr, w=w if w is not None else [out])

    def cp(self, out, in_, eng="dve", r=None, w=None):
        if eng == "act":
            self.op(eng, lambda e: e.copy(out, in_), r=r if r is not None else [in_],
                    w=w if w is not None else [out])
        else:
            self.op(eng, lambda e: e.tensor_copy(out, in_), r=r if r is not None else [in_],
                    w=w if w is not None else [out])

    def memset(self, ap, val, eng="dve", w=None):
        self.op(eng, lambda e: e.memset(ap, val), r=[], w=w if w is not None else [ap])

    def reduce(self, out, in_, op, axis=None, eng="dve", r=None, w=None):
        axis = axis if axis is not None else AX.X
        self.op(eng, lambda e: e.tensor_reduce(out, in_, axis, op),
                r=r if r is not None else [in_], w=w if w is not None else [out])

    def recip(self, out, in_, r=None, w=None):
        self.op("dve", lambda e: e.reciprocal(out, in_), r=r if r is not None else [in_],
                w=w if w is not None else [out])

    def dma(self, out, in_, q="sp", r=(), w=(), **kw):
        self.op(q, lambda e: e.dma_start(out=out, in_=in_, **kw), r=list(r), w=list(w), dma=True)

    def load(self, out, in_, q="sp", **kw):
        self.dma(out, in_, q=q, w=[out], **kw)

    def store(self, out, in_, q="sp", **kw):
        self.dma(out, in_, q=q, r=[in_], **kw)

    def finalize(self):
        nc = self.nc
        ops = self.ops
        engs = ["pe", "act", "dve", "pool", "sp"]
        last_w, readers = {}, {}
        pos = {e: 0 for e in engs}
        dma_idx = 0
        dma_ops = []
        for i, o in enumerate(ops):
            deps = set()
            for k in o["r"]:
                if k in last_w:
                    deps.add(last_w[k])
            for k in o["w"]:
                if k in last_w:
                    deps.add(last_w[k])
                deps.update(readers.get(k, ()))
            for k in o["r"]:
                readers.setdefault(k, []).append(i)
            for k in o["w"]:
                last_w[k] = i
                readers[k] = []
            deps.discard(i)
            if o["dma"]:
                o["didx"] = dma_idx
                if dma_idx >= NDMA_SEMS:
                    deps.add(dma_ops[dma_idx - NDMA_SEMS])
                dma_ops.append(i)
                dma_idx += 1
            else:
                o["pos"] = pos[o["eng"]]
                pos[o["eng"]] += 1
            o["deps"] = deps
        known = {e: {f: -1 for f in engs} for e in engs}
        known_dma = {e: set() for e in engs}
        snap = {}
        signal = set()
        for i, o in enumerate(ops):
            E = o["eng"]
            waits = []
            for d in sorted(o["deps"]):
                od = ops[d]
                if od["dma"]:
                    if d in known_dma[E]:
                        continue
                    known_dma[E].add(d)
                    waits.append(("dma", d))
                else:
                    Fe, p = od["eng"], od["pos"]
                    if Fe == E and (E == "pe" or not SAME_ENGINE_SYNC):
                        continue
                    if known[E][Fe] >= p:
                        continue
                    waits.append(("cmp", d))
                    signal.add(d)
                    known[E][Fe] = p
                    for g, v in snap[d].items():
                        if g != E and known[E][g] < v:
                            known[E][g] = v
            o["waits"] = waits
            if not o["dma"]:
                snap[i] = dict(known[E])
                snap[i][E] = o["pos"]
        cnt = {e: 0 for e in engs}
        for i, o in enumerate(ops):
            if not o["dma"] and i in signal:
                cnt[o["eng"]] += 1
                o["sig"] = cnt[o["eng"]]
        st = self.stack
        sem_c = {e: st.enter_context(nc.semaphore(f"c_{e}")) for e in engs}
        sem_d = [st.enter_context(nc.semaphore(f"d_{j}")) for j in range(NDMA_SEMS)]
        ndma = dma_idx
        block = st.enter_context(nc.Block())
        eng_map = {"pe": block.tensor, "act": block.scalar, "dve": block.vector,
                   "pool": block.gpsimd, "sp": block.sync}

        def emit(E):
            def body(e):
                for i, o in enumerate(ops):
                    if o["eng"] != E:
                        continue
                    for kind, d in o["waits"]:
                        od = ops[d]
                        if kind == "dma":
                            j = od["didx"]
                            e.wait_ge(sem_d[j % NDMA_SEMS], 16 * (j // NDMA_SEMS + 1))
                        else:
                            e.wait_ge(sem_c[od["eng"]], od["sig"])
                    ins = o["fn"](e)
                    if o["dma"]:
                        j = o["didx"]
                        ins.then_inc(sem_d[j % NDMA_SEMS], 16)
                    elif "sig" in o:
                        ins.then_inc(sem_c[E], 1)
                if E == "sp":
                    for j in range(min(NDMA_SEMS, ndma)):
                        n_on = (ndma - 1 - j) // NDMA_SEMS + 1
                        e.wait_ge(sem_d[j], 16 * n_on)
            return body

        for E in engs:
            if E == "sp" or any(o["eng"] == E for o in ops):
                eng_map[E](emit(E))
        st.close()


def run_prog(build, in_maps, n_cores=8):
    nc = bass.Bass("TRN2", target_bir_lowering=False)
    P = Prog(nc)
    build(nc, P)
    P.finalize()
    res = run_bass_kernel_spmd(nc, in_maps, core_ids=list(range(n_cores)))
    return res.results


D = 1024
KC = 8
EPS = 1e-6


def dram_in(nc, name, shape, dt=F32):
    return nc.dram_tensor(name, list(shape), dt, kind="ExternalInput").ap()


def dram_out(nc, name, shape, dt=F32):
    return nc.dram_tensor(name, list(shape), dt, kind="ExternalOutput").ap()


def emit_consts(P):
    ones = P.sb([128, 128], F32, "ones_f")
    P.memset(ones[:], 1.0)
    return dict(ones=ones)


def emit_mod(P, cvec, modw, modbT, ncol=2, wbuf=None):
    craw = P.sb([128, KC, ncol], F32)
    sc = P.sb([128, KC, ncol], F32)
    P.load(craw[:], cvec.rearrange("(k p) n -> p k n", p=128))
    P.act(sc[:], craw[:], AF.Silu)
    bT = P.sb([128, 48], F32)
    P.load(bT[:], modbT)
    mp = P.ps([128, 48, ncol], F32)
    if wbuf is None:
        wbuf = [P.sb([128, KC, 512], F32) for _ in range(2)]
    mw = modw.rearrange("(k p) n -> p k n", p=128)
    for j in range(12):
        wb = wbuf[j % 2]
        P.load(wb[:], mw[:, :, j * 512:(j + 1) * 512], q="sp" if j % 2 == 0 else "act")
        for jj in range(4):
            o = j * 4 + jj
            for k in range(KC):
                P.mm(mp[:, o, :], wb[:, k, jj * 128:(jj + 1) * 128], sc[:, k, :],
                     start=(k == 0), stop=(k == KC - 1))
    mT = P.sb([128, 48, ncol], F32)
    for n in range(ncol):
        P.tt(mT[:, :, n], mp[:, :, n], bT[:], ALU.add)
    return mT


def emit_geff(P, mT, gT, shift_part, scale_part, ncol=2):
    geff = P.sb([128, KC, ncol], F32)
    for n in range(ncol):
        P.stt(geff[:, :, n], mT[:, scale_part * 8:(scale_part + 1) * 8, n], 1.0, gT[:], ALU.add, ALU.mult)
    return geff


def emit_rstd(P, C, xv, n, x2v, rs_ps_v, rstd_v):
    P.act(x2v, xv, AF.Square)
    for k in range(KC):
        P.mm(rs_ps_v, C["ones"][:], x2v[:, k, :], start=(k == 0), stop=(k == KC - 1))
    P.act(rstd_v, rs_ps_v, AF.Sqrt, bias=C["eps"][:, 0:1], scale=1.0 / D)
    P.recip(rstd_v, rstd_v)


def emit_normmod(P, xv, n, rstd_v, geff, mT, shift_part, col, outv):
    for k in range(KC):
        tmp = outv[:, k, :]
        P.stt(tmp, xv[:, k, :], geff[:, k, col:col + 1], rstd_v, ALU.mult, ALU.mult)
        P.act(tmp, tmp, AF.Identity, bias=mT[:, shift_part * 8 + k, col:col + 1], scale=1.0)


TA = 4224
HYB_IN = 2304


def token_blocks(T_lat, T_ctx):
    blks = []
    t = 0
    while t < T_lat:
        blks.append((t, 512, 0))
        t += 512
    if T_ctx:
        blks.append((T_lat, T_ctx, 1))
    return blks


def build_stage_a(nc, P):
    xT = dram_in(nc, "xT", [D, TA])
    cvec = dram_in(nc, "cvec", [D, 2])
    modw = dram_in(nc, "modw", [D, 6 * D])
    modbT = dram_in(nc, "modbT", [128, 48])
    g1T = dram_in(nc, "g1T", [128, KC])
    w_in = dram_in(nc, "w_in", [D, HYB_IN])
    pT = dram_out(nc, "pT", [HYB_IN, TA])

    C = emit_consts(P)
    eps = P.sb([128, 1], F32)
    P.memset(eps[:], EPS)
    C["eps"] = eps
    wbf = P.sb([128, KC, HYB_IN], BF16, "w_in_bf")
    w_v = w_in.rearrange("(k p) n -> p k n", p=128)
    for k in range(KC):
        P.load(wbf[:, k, :], w_v[:, k, :], q="pool")
    gT = P.sb([128, KC], F32)
    P.load(gT[:], g1T)
    mT = emit_mod(P, cvec, modw, modbT)
    geff = emit_geff(P, mT, gT, 0, 1)

    xv = xT.rearrange("(k p) t -> p k t", p=128)
    xb = [P.sb([128, KC, 512], F32) for _ in range(2)]
    x2 = P.sb([128, KC, 512], F32)
    rs_ps = P.ps([128, 512], F32)
    rstd = P.sb([128, 512], F32)
    ab = [P.sb([128, KC, 512], BF16) for _ in range(2)]
    pps = [P.ps([128, 512], F32) for _ in range(4)]
    ost = [P.sb([128, 6, 512], F32) for _ in range(2)]
    NM = HYB_IN // 128
    oi = 0
    for bi, (t0, n, col) in enumerate(token_blocks(4096, 128)):
        xt = xb[bi % 2]
        a = ab[bi % 2]
        P.load(xt[:, :, :n], xv[:, :, t0:t0 + n], q="sp")
        emit_rstd(P, C, xt[:, :, :n], n, x2[:, :, :n], rs_ps[:, :n], rstd[:, :n])
        emit_normmod(P, xt[:, :, :n], n, rstd[:, :n], geff, mT, 0, col, a[:, :, :n])
        for m0 in range(0, NM, 6):
            o = ost[oi % 2]
            oi += 1
            for mi in range(6):
                m = m0 + mi
                pp = pps[m % 4]
                for k in range(KC):
                    P.mm(pp[:, :n], wbf[:, k, m * 128:(m + 1) * 128], a[:, k, :n],
                         start=(k == 0), stop=(k == KC - 1))
                if m % 2 == 0:
                    P.cp(o[:, mi, :n], pp[:, :n], eng="dve")
                else:
                    P.cp(o[:, mi, :n], pp[:, :n], eng="act")
            P.store(pT[m0 * 128:(m0 + 6) * 128, t0:t0 + n].rearrange("(m p) t -> p m t", p=128),
                    o[:, :, :n], q="act")


NEXP = 16
DEXP = 256
BIG = 1.0e4


def build_stage_tail(last, T_lat, T_ctx):
    T = T_lat + T_ctx
    blocks = token_blocks(T_lat, T_ctx)
    parts = [blocks[i:i + 2] for i in range(0, 8, 2)]
    if T_ctx:
        parts[-1] = parts[-1] + [blocks[8]]
    PT = max(sum(n for _, n, _ in p) for p in parts)

    def build(nc, P):
        catT = dram_in(nc, "catT", [D, T])
        hT = dram_in(nc, "hT", [D, T])
        cvec = dram_in(nc, "cvec", [D, 2])
        modw = dram_in(nc, "modw", [D, 6 * D])
        modbT = dram_in(nc, "modbT", [128, 48])
        g2T = dram_in(nc, "g2T", [128, KC])
        gfT = dram_in(nc, "gfT", [128, KC])
        w_out = dram_in(nc, "w_out", [D, D])
        rw = dram_in(nc, "rw", [D, NEXP])
        rbb = dram_in(nc, "rbb", [128, NEXP])
        exg = dram_in(nc, "exg", [NEXP + 1, D, DEXP])
        exu = dram_in(nc, "exu", [NEXP + 1, D, DEXP])
        exd = dram_in(nc, "exd", [NEXP + 1, DEXP, D])
        ident_d = dram_in(nc, "ident", [128, 128])
        sel_d = dram_in(nc, "sel", [NEXP, NEXP * 128])
        houtT = dram_out(nc, "houtT", [D, T])

        C = emit_consts(P)
        eps = P.sb([128, 1], F32)
        P.memset(eps[:], EPS)
        C["eps"] = eps
        ident = P.sb([128, 128], F32)
        P.load(ident[:], ident_d)
        sel = P.sb([NEXP, NEXP * 128], F32)
        P.load(sel[:], sel_d)
        rws = P.sb([128, KC, NEXP], F32)
        P.load(rws[:], rw.rearrange("(k p) e -> p k e", p=128))
        rbs = P.sb([128, NEXP], F32)
        P.load(rbs[:], rbb)
        gT = P.sb([128, KC], F32)
        P.load(gT[:], g2T)
        gf = P.sb([128, KC], F32)
        P.load(gf[:], gfT)
        wo = P.sb([128, KC, D], BF16, "wo_bf")
        wo_v = w_out.rearrange("(k p) n -> p k n", p=128)
        for k in range(KC):
            P.load(wo[:, k, :], wo_v[:, k, :], q="pool")
        fb = [P.sb([128, KC, 512], F32, f"fblk{i}") for i in range(3)]
        mT = emit_mod(P, cvec, modw, modbT, wbuf=fb[:2])
        geff = emit_geff(P, mT, gT, 3, 4)

        h = P.sb([128, KC, PT], F32, "h_res")
        fT = P.sb([128, KC, PT], BF16, "f_bf")
        combT = P.sb([NEXP, PT], F32, "combT")
        catbf = P.sb([128, KC, 512], BF16, "catbf")
        rstd = P.sb([128, 512], F32, "rstd")
        B = [P.ps([128, 512], F32, f"bank{i}") for i in range(7)]
        wg = [P.sb([128, KC, DEXP], BF16, f"wg{i}") for i in range(2)]
        wu = [P.sb([128, KC, DEXP], BF16, f"wu{i}") for i in range(2)]
        wd = [P.sb([128, 2, D], BF16, f"wd{i}") for i in range(2)]
        sil = [P.sb([128, 512], F32, f"sil{i}") for i in range(2)]
        tmpu = [P.sb([128, 512], F32, f"tmpu{i}") for i in range(2)]
        hid = [P.sb([128, 2, 512], BF16, f"hid{i}") for i in range(2)]
        R = {nm: P.sb([128, NEXP], F32, "r_" + nm) for nm in ("sc", "bi", "eq1", "b2", "eq2", "sel", "ws", "cb")}
        R4 = {nm: P.sb([128, 4], F32, "r4_" + nm) for nm in ("m1", "m2", "gs", "gsel")}
        R1 = {nm: P.sb([128, 1], F32, "r1_" + nm) for nm in ("gmax", "den", "rden")}
        catv = catT.rearrange("(k p) t -> p k t", p=128)
        hv = hT.rearrange("(k p) t -> p k t", p=128)
        hov = houtT.rearrange("(k p) t -> p k t", p=128)
        widx = 0
        for part in parts:
            lt = 0
            locs = []
            for (t0, n, col) in part:
                locs.append((t0, n, col, lt))
                cb_, x2_, f32_ = fb[0], fb[1], fb[2]
                P.load(cb_[:, :, :n], catv[:, :, t0:t0 + n], q="sp")
                P.load(h[:, :, lt:lt + n], hv[:, :, t0:t0 + n], q="act")
                P.cp(catbf[:, :, :n], cb_[:, :, :n], eng="act")
                for m in range(KC):
                    pp = B[m % 2]
                    for k in range(KC):
                        P.mm(pp[:, :n], wo[:, k, m * 128:(m + 1) * 128], catbf[:, k, :n],
                             start=(k == 0), stop=(k == KC - 1))
                    P.stt(h[:, m, lt:lt + n], pp[:, :n], mT[:, 16 + m, col:col + 1], h[:, m, lt:lt + n],
                          ALU.mult, ALU.add)
                emit_rstd(P, C, h[:, :, lt:lt + n], n, x2_[:, :, :n], B[2][:, :n], rstd[:, :n])
                emit_normmod(P, h[:, :, lt:lt + n], n, rstd[:, :n], geff, mT, 3, col, f32_[:, :, :n])
                P.cp(fT[:, :, lt:lt + n], f32_[:, :, :n], eng="act")
                for tt in range(n // 128):
                    lg = B[3]
                    for k in range(KC):
                        P.mm(lg[:, :NEXP], f32_[:, k, tt * 128:(tt + 1) * 128], rws[:, k, :],
                             start=(k == 0), stop=(k == KC - 1))
                    sc, bi, eq1, b2, eq2, sl, ws, cb = (R[x] for x in ("sc", "bi", "eq1", "b2", "eq2", "sel", "ws", "cb"))
                    m1, m2, gs, gsel = (R4[x] for x in ("m1", "m2", "gs", "gsel"))
                    P.act(sc[:], lg[:, :NEXP], AF.Sigmoid)
                    P.tt(bi[:], sc[:], rbs[:], ALU.add)
                    for g in range(4):
                        P.reduce(m1[:, g:g + 1], bi[:, 4 * g:4 * g + 4], ALU.max)
                    for g in range(4):
                        P.ts(eq1[:, 4 * g:4 * g + 4], bi[:, 4 * g:4 * g + 4], m1[:, g:g + 1], None, ALU.is_equal)
                    P.stt(b2[:], eq1[:], -BIG, bi[:], ALU.mult, ALU.add)
                    for g in range(4):
                        P.reduce(m2[:, g:g + 1], b2[:, 4 * g:4 * g + 4], ALU.max)
                    P.tt(gs[:], m1[:], m2[:], ALU.add)
                    P.reduce(R1["gmax"][:], gs[:], ALU.max)
                    P.ts(gsel[:], gs[:], R1["gmax"][:, 0:1], None, ALU.is_equal)
                    for g in range(4):
                        P.ts(eq2[:, 4 * g:4 * g + 4], bi[:, 4 * g:4 * g + 4], m2[:, g:g + 1], None, ALU.is_equal)
                    P.tt(sl[:], eq1[:], eq2[:], ALU.add)
                    for g in range(4):
                        P.ts(sl[:, 4 * g:4 * g + 4], sl[:, 4 * g:4 * g + 4], gsel[:, g:g + 1], None, ALU.mult)
                    P.tt(ws[:], sl[:], sc[:], ALU.mult)
                    P.reduce(R1["den"][:], ws[:], ALU.add)
                    P.recip(R1["rden"][:], R1["den"][:])
                    P.ts(cb[:], ws[:], R1["rden"][:, 0:1], None, ALU.mult)
                    P.tr(B[4][:NEXP, :128], cb[:, :NEXP], ident[:])
                    P.cp(combT[:, lt + tt * 128:lt + (tt + 1) * 128], B[4][:NEXP, :128], eng="act")
                lt += n
            for e in range(NEXP + 1):
                wi = widx % 2
                widx += 1
                P.load(wg[wi][:], exg[e].rearrange("(k p) f -> p k f", p=128), q="pool")
                P.load(wu[wi][:], exu[e].rearrange("(k p) f -> p k f", p=128), q="pool")
                P.load(wd[wi][:], exd[e].rearrange("(k p) n -> p k n", p=128), q="pool")
                for (t0, n, col, l0) in locs:
                    if e < NEXP:
                        P.mm(B[4][:, :n], sel[:, e * 128:(e + 1) * 128], combT[:, l0:l0 + n])
                    hh = hid[0]
                    for hc in range(2):
                        gp, up = B[hc], B[2 + hc]
                        for k in range(KC):
                            P.mm(gp[:, :n], wg[wi][:, k, hc * 128:(hc + 1) * 128], fT[:, k, l0:l0 + n],
                                 start=(k == 0), stop=(k == KC - 1))
                        for k in range(KC):
                            P.mm(up[:, :n], wu[wi][:, k, hc * 128:(hc + 1) * 128], fT[:, k, l0:l0 + n],
                                 start=(k == 0), stop=(k == KC - 1))
                        P.act(sil[hc][:, :n], gp[:, :n], AF.Silu)
                        if e < NEXP:
                            P.tt(tmpu[hc][:, :n], up[:, :n], sil[hc][:, :n], ALU.mult)
                            P.tt(hh[:, hc, :n], tmpu[hc][:, :n], B[4][:, :n], ALU.mult)
                        else:
                            P.tt(hh[:, hc, :n], up[:, :n], sil[hc][:, :n], ALU.mult)
                    for m in range(KC):
                        dp = B[5 + (m % 2)]
                        for hc in range(2):
                            P.mm(dp[:, :n], wd[wi][:, hc, m * 128:(m + 1) * 128], hh[:, hc, :n],
                                 start=(hc == 0), stop=(hc == 1))
                        P.stt(h[:, m, l0:l0 + n], dp[:, :n], mT[:, 40 + m, col:col + 1], h[:, m, l0:l0 + n],
                              ALU.mult, ALU.add)
            for (t0, n, col, l0) in locs:
                if last:
                    emit_rstd(P, C, h[:, :, l0:l0 + n], n, fb[1][:, :, :n], B[2][:, :n], rstd[:, :n])
                    for k in range(KC):
                        P.stt(fb[0][:, k, :n], h[:, k, l0:l0 + n], gf[:, k:k + 1], rstd[:, :n], ALU.mult, ALU.mult)
                    P.store(hov[:, :, t0:t0 + n], fb[0][:, :, :n], q="sp")
                else:
                    P.store(hov[:, :, t0:t0 + n], h[:, :, l0:l0 + n], q="sp")
    return build


NKEY = 8448
NQ = 4096
NH = 16
MLA_SCALE = 96.0 ** -0.5
QR = 112


def build_stage_d():
    def build(nc, P):
        hT = dram_in(nc, "hT", [D, NKEY])
        hqT = dram_in(nc, "hqT", [D, NQ])
        cosq_d = dram_in(nc, "cosq", [128, NQ])
        sinq_d = dram_in(nc, "sinq", [128, NQ])
        cvec = dram_in(nc, "cvec", [D, 2])
        modw = dram_in(nc, "modw", [D, 6 * D])
        modbT = dram_in(nc, "modbT", [128, 48])
        g1T = dram_in(nc, "g1T", [128, KC])
        win_d = dram_in(nc, "win", [D, 384])
        wka_d = dram_in(nc, "wka", [D, QR])
        wkb_d = dram_in(nc, "wkb", [D, QR])
        wqa_d = dram_in(nc, "wqa", [256, NH * QR])
        wqb_d = dram_in(nc, "wqb", [256, NH * QR])
        kupk_d = dram_in(nc, "kupk", [128, NH * 64])
        kupv_d = dram_in(nc, "kupv", [128, NH * 64])
        gq_d = dram_in(nc, "gq", [128, 2])
        gkv_d = dram_in(nc, "gkv", [128, 1])
        cos_d = dram_in(nc, "cosT", [128, NKEY])
        sin_d = dram_in(nc, "sinT", [128, NKEY])
        attT = dram_out(nc, "attT", [NH * 64, NQ])

        C = emit_consts(P)
        eps = P.sb([128, 1], F32)
        P.memset(eps[:], EPS)
        C["eps"] = eps
        onesb = P.sb([128, 64], F32, "onesb")
        P.memset(onesb[:], 1.0)
        gT = P.sb([128, KC], F32)
        P.load(gT[:], g1T)
        gq = P.sb([128, 2], F32)
        P.load(gq[:], gq_d)
        gkv = P.sb([128, 1], F32)
        P.load(gkv[:], gkv_d)
        win = P.sb([128, KC, 384], BF16, "win")
        P.load(win[:], win_d.rearrange("(k p) n -> p k n", p=128), q="pool")
        wka = P.sb([128, KC, QR], BF16, "wka")
        P.load(wka[:], wka_d.rearrange("(k p) n -> p k n", p=128), q="pool")
        wkb = P.sb([128, KC, QR], BF16, "wkb")
        P.load(wkb[:], wkb_d.rearrange("(k p) n -> p k n", p=128), q="pool")
        wqa = P.sb([128, 2, NH * QR], BF16, "wqa")
        P.load(wqa[:], wqa_d.rearrange("(k p) n -> p k n", p=128), q="pool")
        wqb = P.sb([128, 2, NH * QR], BF16, "wqb")
        P.load(wqb[:], wqb_d.rearrange("(k p) n -> p k n", p=128), q="pool")
        kupk = P.sb([128, NH * 64], BF16, "kupk")
        P.load(kupk[:], kupk_d, q="pool")
        kupv = P.sb([128, NH * 64], BF16, "kupv")
        P.load(kupv[:], kupv_d, q="pool")

        hb = P.sb([128, KC, 512], F32, "hblk")
        x2 = P.sb([128, KC, 512], F32, "x2blk")
        mT = emit_mod(P, cvec, modw, modbT, wbuf=[hb, x2])
        geff = emit_geff(P, mT, gT, 0, 1)

        B = [P.ps([128, 512], F32, f"bank{i}") for i in range(7)]
        rstd = P.sb([128, 512], F32, "rstd")
        ab = P.sb([128, KC, 512], BF16, "a_bf")
        kvn = P.sb([128, NKEY], BF16, "kvn")
        kpe = P.sb([128, NKEY], BF16, "kpe")
        qn = P.sb([128, 2, NQ], BF16, "qn")
        kvd = P.sb([128, 512], F32, "kvd")
        kv2 = P.sb([128, 512], F32, "kv2")
        qd = P.sb([128, 2, 512], F32, "qd")
        qd2 = P.sb([128, 2, 512], F32, "qd2")
        rs2 = P.sb([128, 512], F32, "rs2")
        cosb = P.sb([128, 512], F32, "cosb")
        sinb = P.sb([128, 512], F32, "sinb")
        t1 = P.sb([128, 512], F32, "t1")
        t2 = P.sb([128, 512], F32, "t2")
        hv = hT.rearrange("(k p) t -> p k t", p=128)
        blocks = token_blocks(8192, 256)
        blocks[-1] = (8192, 256, 1)

        def rope(dst, Aps, Bps, n):
            for (lo, sgn_first) in ((64, 0), (96, 1)):
                sl = slice(lo, lo + 16)
                ca, cb_ = (cosb, sinb) if sgn_first == 0 else (sinb, cosb)
                P.tt(t1[sl, :n], Aps[sl, :n], ca[sl, :n], ALU.mult)
                P.tt(t2[sl, :n], Bps[sl, :n], cb_[sl, :n], ALU.mult)
                P.tt(dst(sl), t1[sl, :n], t2[sl, :n], ALU.subtract if sgn_first == 0 else ALU.add)

        for (t0, n, col) in blocks:
            P.load(hb[:, :, :n], hv[:, :, t0:t0 + n], q="sp")
            P.load(cosb[:, :n], cos_d[:, t0:t0 + n], q="act")
            P.load(sinb[:, :n], sin_d[:, t0:t0 + n], q="act")
            emit_rstd(P, C, hb[:, :, :n], n, x2[:, :, :n], B[0][:, :n], rstd[:, :n])
            emit_normmod(P, hb[:, :, :n], n, rstd[:, :n], geff, mT, 0, col, ab[:, :, :n])
            for k in range(KC):
                P.mm(B[1][:, :n], win[:, k, 256:384], ab[:, k, :n], start=(k == 0), stop=(k == KC - 1))
            P.cp(kvd[:, :n], B[1][:, :n], eng="act")
            P.act(kv2[:, :n], kvd[:, :n], AF.Square)
            P.mm(B[2][:, :n], C["ones"][:], kv2[:, :n])
            P.act(rs2[:, :n], B[2][:, :n], AF.Sqrt, bias=eps[:, 0:1], scale=1.0 / 128)
            P.recip(rs2[:, :n], rs2[:, :n])
            P.stt(kvn[:, t0:t0 + n], kvd[:, :n], gkv[:, 0:1], rs2[:, :n], ALU.mult, ALU.mult)
            for k in range(KC):
                P.mm(B[3][:QR, :n], wka[:, k, :], ab[:, k, :n], start=(k == 0), stop=(k == KC - 1))
            for k in range(KC):
                P.mm(B[4][:QR, :n], wkb[:, k, :], ab[:, k, :n], start=(k == 0), stop=(k == KC - 1))
            rope(lambda sl, t0=t0, n=n: kpe[sl, t0:t0 + n], B[3], B[4], n)
        hqv = hqT.rearrange("(k p) t -> p k t", p=128)
        for q0 in range(0, NQ, 512):
            n = 512
            P.load(hb[:, :, :n], hqv[:, :, q0:q0 + n], q="sp")
            emit_rstd(P, C, hb[:, :, :n], n, x2[:, :, :n], B[0][:, :n], rstd[:, :n])
            emit_normmod(P, hb[:, :, :n], n, rstd[:, :n], geff, mT, 0, 0, ab[:, :, :n])
            for c2 in range(2):
                for k in range(KC):
                    P.mm(B[5 + c2][:, :n], win[:, k, c2 * 128:(c2 + 1) * 128], ab[:, k, :n],
                         start=(k == 0), stop=(k == KC - 1))
                P.cp(qd[:, c2, :n], B[5 + c2][:, :n], eng="act")
            P.act(qd2[:, :, :n], qd[:, :, :n], AF.Square)
            for c2 in range(2):
                P.mm(B[2][:, :n], C["ones"][:], qd2[:, c2, :n], start=(c2 == 0), stop=(c2 == 1))
            P.act(rs2[:, :n], B[2][:, :n], AF.Sqrt, bias=eps[:, 0:1], scale=1.0 / 256)
            P.recip(rs2[:, :n], rs2[:, :n])
            for c2 in range(2):
                P.stt(qn[:, c2, q0:q0 + n], qd[:, c2, :n], gq[:, c2:c2 + 1], rs2[:, :n], ALU.mult, ALU.mult)

        NKT = NKEY // 128
        kT = P.sb([128, NKEY], BF16, "kT_h")
        vh = P.sb([128, NKT, 65], BF16, "v_h")
        P.memset(kT[:], 0.0, eng="pool")
        P.memset(vh[:], 1.0, eng="pool")
        qT = [P.sb([128, 512], BF16, f"qT{i}") for i in range(2)]
        for i in range(2):
            P.memset(qT[i][:], 0.0, eng="pool")
        pT = [P.sb([128, 512], BF16, f"pT{i}") for i in range(3)]
        rden = P.sb([128, 512], F32, "rden")
        ost = [P.sb([64, 512], F32, f"ost{i}") for i in range(2)]
        SB_, OB, QA, QB, BC = (B[0], B[1]), (B[2], B[3]), B[4], B[5], B[6]
        cos_q = cosq_d
        sin_q = sinq_d
        pi = 0
        qi = 0
        for h in range(NH):
            for j in range(0, NKEY, 512):
                n = min(512, NKEY - j)
                pp = SB_[(j // 512) % 2]
                P.mm(pp[:64, :n], kupk[:, h * 64:(h + 1) * 64], kvn[:, j:j + n])
                P.cp(kT[0:64, j:j + n], pp[:64, :n], eng="dve" if (j // 512) % 2 == 0 else "act")
            P.cp(kT[64:80, :], kpe[64:80, :], eng="pool")
            P.cp(kT[96:112, :], kpe[96:112, :], eng="pool")
            for k0 in range(0, NKT, 8):
                nk = min(8, NKT - k0)
                pp = OB[(k0 // 8) % 2]
                for kk in range(nk):
                    P.mm(pp[:, kk * 64:(kk + 1) * 64], kvn[:, (k0 + kk) * 128:(k0 + kk + 1) * 128],
                         kupv[:, h * 64:(h + 1) * 64])
                P.cp(vh[:, k0:k0 + nk, 0:64], pp[:, :nk * 64].rearrange("p (a b) -> p a b", b=64), eng="dve")
            for qt in range(NQ // 512):
                q0 = qt * 512
                qq = qT[qi % 2]
                oo = OB[qi % 2]
                os_ = ost[qi % 2]
                qi += 1
                for c2 in range(2):
                    P.mm(QA[:QR, :], wqa[:, c2, h * QR:(h + 1) * QR], qn[:, c2, q0:q0 + 512],
                         start=(c2 == 0), stop=(c2 == 1))
                for c2 in range(2):
                    P.mm(QB[:QR, :], wqb[:, c2, h * QR:(h + 1) * QR], qn[:, c2, q0:q0 + 512],
                         start=(c2 == 0), stop=(c2 == 1))
                P.load(cosb[:, :], cos_q[:, q0:q0 + 512], q="sp")
                P.load(sinb[:, :], sin_q[:, q0:q0 + 512], q="sp")
                P.cp(qq[0:64, :], QA[0:64, :], eng="act")
                rope(lambda sl, qq=qq: qq[sl, :], QA, QB, 512)
                for kt in range(NKT):
                    sp_ = SB_[kt % 2]
                    pt = pT[pi % 3]
                    pi += 1
                    P.mm(sp_[:, :], kT[0:QR, kt * 128:(kt + 1) * 128], qq[0:QR, :])
                    P.act(pt[:, :], sp_[:, :], AF.Exp, scale=MLA_SCALE)
                    P.mm(oo[:65, :], vh[:, kt, :], pt[:, :], start=(kt == 0), stop=(kt == NKT - 1))
                P.recip(rden[64:65, :], oo[64:65, :])
                P.mm(BC[:64, :], onesb[64:65, :], rden[64:65, :])
                P.cp(os_[:, :], oo[:64, :], eng="act")
                P.tt(os_[:, :], os_[:, :], BC[:64, :], ALU.mult)
                P.store(attT[h * 64:(h + 1) * 64, q0:q0 + 512], os_[:, :], q="act")
    return build


TB = 8448
CH = 64
NCHUNK = TB // CH
DECAY_SCALE = math.exp(-0.5)


def rwkv_blocks():
    blks = [(t, 512, 0, 8192) for t in range(0, 8192, 512)]
    blks.append((8192, 256, 8192, 8448))
    return blks


def emit_rwkv_scan(nc, P, scr, gC_o, F):
    ident_d = dram_in(nc, "ident64", [64, 64])
    masks_d = dram_in(nc, "masks", [64, 3 * 512])
    yT = dram_out(nc, "yT", [2, 4, 64, TB])
    ident = P.sb([64, 64], BF16, "ident64")
    P.load(ident[:], ident_d, q="pool")
    masks = P.sb([64, 3, 512], F32, "masks")
    P.load(masks[:], masks_d.rearrange("p (m c) -> p m c", m=3))
    mATs, mATi, mL = masks[:, 0, :], masks[:, 1, :], masks[:, 2, :]
    gC64 = P.sb([64, 8, NCHUNK], F32, "gC64")
    for z in range(2):
        for hp in range(2):
            for hh in range(2):
                hd = z * 4 + hp * 2 + hh
                P.dma(gC64[:, hd, :], gC_o[z * 2 + hp, hh * 64:(hh + 1) * 64, :], q="sp",
                      r=[("gC", z * 2 + hp)], w=[gC64])
    Tb = [P.ps([128, 1024], BF16, f"tbank{i}") for i in range(2)]
    dbuf = [P.sb([64, 8, 5, CH], BF16, f"dbuf{i}") for i in range(3)]
    W2 = 8 * CH
    Nm = [P.sb([64, W2], BF16, f"Nm{i}") for i in range(2)]
    Mm = [P.sb([64, W2], BF16, f"Mm{i}") for i in range(2)]
    ATkk = P.sb([64, W2], BF16, "ATkk")
    ATrb = [P.sb([64, W2], BF16, f"ATrb{i}") for i in range(2)]
    ATrk = [P.sb([64, W2], BF16, f"ATrk{i}") for i in range(2)]
    Tt = [P.sb([64, 4, W2], BF16, f"Tt{i}") for i in range(2)]
    X32 = P.sb([64, 8, 128], F32, "X32")
    Xbf = P.sb([64, 8, 128], BF16, "Xbf")
    Wf = [P.sb([64, W2], BF16, f"Wf{i}") for i in range(2)]
    U0s = [P.sb([64, 8, CH], F32, f"U0s{i}") for i in range(2)]
    Ubf = P.sb([64, W2], BF16, "Ubf")
    P32 = P.sb([64, W2], F32, "P32")
    Pbf = P.sb([64, W2], BF16, "Pbf")
    ptmp = P.sb([64, W2], F32, "ptmp")
    yst = [P.sb([64, 8, CH], F32, f"yst{i}") for i in range(2)]
    P.memset(P32[:], 0.0)
    P.memset(Pbf[:], 0.0)
    fi = [0]

    def bank():
        b = F[fi[0] % len(F)]
        fi[0] += 1
        return b
    order = [[128, 129, 130, 131] + list(range(128)), [131, 130, 129, 128] + list(range(127, -1, -1))]
    blk_of = lambda tok: (tok // 512) if tok < 8192 else 16
    hs = lambda hd: slice(hd * CH, (hd + 1) * CH)
    for j in range(NCHUNK):
        d = dbuf[j % 3]
        par = j % 2
        toks = [order[0][j] * CH, order[1][j] * CH]
        for z in range(2):
            for hp in range(2):
                for hh in range(2):
                    hd = z * 4 + hp * 2 + hh
                    P.dma(d[:, hd, :, :],
                          scr[z, hp, :, hh * 64:(hh + 1) * 64, toks[z]:toks[z] + CH].rearrange("a k t -> k a t"),
                          q="sp" if hd % 2 == 0 else "act", r=[("scr", z, hp, blk_of(toks[z]))], w=[d])
        rf = lambda hd: d[:, hd, 0, :]
        kapf = lambda hd: d[:, hd, 1, :]
        bf_ = lambda hd: d[:, hd, 2, :]
        kf = lambda hd: d[:, hd, 3, :]
        vf = lambda hd: d[:, hd, 4, :]
        pLT, pL, pkk, prb, prk = bank(), bank(), bank(), bank(), bank()
        for hd in range(8):
            P.mm(pLT[:64, hs(hd)], bf_(hd), kapf(hd))
        for hd in range(8):
            P.mm(pL[:64, hs(hd)], kapf(hd), bf_(hd))
        for hd in range(8):
            P.mm(pkk[:64, hs(hd)], kf(hd), kapf(hd))
        for hd in range(8):
            P.mm(prb[:64, hs(hd)], bf_(hd), rf(hd))
        for hd in range(8):
            P.mm(prk[:64, hs(hd)], kf(hd), rf(hd))
        N, M = Nm[0], Mm[0]
        P.stt(N[:], pLT[:64, :], -1.0, mATs, ALU.mult, ALU.mult)
        P.stt(M[:], pL[:64, :], -1.0, mL, ALU.mult, ALU.mult)
        P.tt(ATkk[:], pkk[:64, :], mATs, ALU.mult)
        P.tt(ATrb[par][:], prb[:64, :], mATi, ALU.mult)
        P.tt(ATrk[par][:], prk[:64, :], mATi, ALU.mult)
        T = Tt[par]
        for ai, src_f in enumerate((kapf, bf_, kf, vf)):
            tb = Tb[ai % 2]
            for hd in range(8):
                P.tr(tb[:64, hs(hd)], src_f(hd), ident[:])
            P.cp(T[:, ai, :], tb[:64, :W2], eng="act")
        pav = bank()
        for hd in range(8):
            P.mm(pav[:64, hs(hd)], ATkk[:, hs(hd)], T[:, 3, hs(hd)])
        P.cp(X32[:, :, 0:CH], T[:, 0, :].rearrange("p (a b) -> p a b", b=CH), eng="act")
        P.cp(X32[:, :, CH:2 * CH], pav[:64, :].rearrange("p (a b) -> p a b", b=CH), eng="dve")
        P.cp(Xbf[:], X32[:], eng="act")
        for lvl in range(6):
            N, M = Nm[lvl % 2], Mm[lvl % 2]
            pa, pb = bank(), bank()
            for hd in range(8):
                pp = pa if hd < 4 else pb
                P.mm(pp[:64, (hd % 4) * 128:(hd % 4 + 1) * 128], N[:, hs(hd)], Xbf[:, hd, :])
            if lvl < 5:
                N2, M2 = Nm[(lvl + 1) % 2], Mm[(lvl + 1) % 2]
                pn = bank()
                for hd in range(8):
                    P.mm(pn[:64, hs(hd)], M[:, hs(hd)], N[:, hs(hd)])
                if lvl < 4:
                    pm = bank()
                    for hd in range(8):
                        P.mm(pm[:64, hs(hd)], N[:, hs(hd)], M[:, hs(hd)])
            P.tt(X32[:, 0:4, :], X32[:, 0:4, :], pa[:64, :].rearrange("p (a b) -> p a b", b=128), ALU.add)
            P.tt(X32[:, 4:8, :], X32[:, 4:8, :], pb[:64, :].rearrange("p (a b) -> p a b", b=128), ALU.add)
            P.cp(Xbf[:], X32[:], eng="act")
            if lvl < 5:
                P.cp(N2[:], pn[:64, :], eng="act")
                if lvl < 4:
                    P.cp(M2[:], pm[:64, :], eng="dve")
        tb = Tb[0]
        for hd in range(8):
            P.tr(tb[:64, hs(hd)], Xbf[:, hd, 0:CH], ident[:])
        P.cp(Wf[par][:], tb[:64, :W2], eng="act")
        P.cp(U0s[par][:], X32[:, :, CH:2 * CH], eng="dve")
        pu = bank()
        for hd in range(8):
            P.mm(pu[:64, hs(hd)], Wf[par][:, hs(hd)], Pbf[:, hs(hd)])
        P.stt(Ubf[:].rearrange("p (a b) -> p a b", b=CH), pu[:64, :].rearrange("p (a b) -> p a b", b=CH), -1.0,
              U0s[par][:], ALU.mult, ALU.subtract)
        py = bank()
        for hd in range(8):
            P.mm(py[:64, hs(hd)], Pbf[:, hs(hd)], rf(hd), start=True, stop=False)
            P.mm(py[:64, hs(hd)], Ubf[:, hs(hd)], ATrb[par][:, hs(hd)], start=False, stop=False)
            P.mm(py[:64, hs(hd)], T[:, 3, hs(hd)], ATrk[par][:, hs(hd)], start=False, stop=True)
        ys = yst[par]
        P.cp(ys[:], py[:64, :].rearrange("p (a b) -> p a b", b=CH), eng="act")
        for z in range(2):
            P.store(yT[z, :, :, toks[z]:toks[z] + CH].rearrange("h v t -> v h t"), ys[:, z * 4:(z + 1) * 4, :],
                    q="act")
        pd = bank()
        for hd in range(8):
            P.mm(pd[:64, hs(hd)], T[:, 1, hs(hd)], Ubf[:, hs(hd)], start=True, stop=False)
            P.mm(pd[:64, hs(hd)], T[:, 2, hs(hd)], T[:, 3, hs(hd)], start=False, stop=True)
        P.tt(ptmp[:], pd[:64, :], P32[:], ALU.add)
        for hd in range(8):
            ch = order[hd // 4][j]
            P.ts(P32[:, hs(hd)], ptmp[:, hs(hd)], gC64[:, hd, ch:ch + 1], None, ALU.mult)
        P.cp(Pbf[:], P32[:], eng="act")


def build_stage_b(phase_s=True):
    def build(nc, P):
        pin = dram_in(nc, "pin", [1024, TB])
        cw_d = dram_in(nc, "cw", [128, 24])
        wup_d = dram_in(nc, "wup", [64, 512])
        aup_d = dram_in(nc, "aup", [64, 512])
        gup_d = dram_in(nc, "gup", [128, 256])
        w0bc_d = dram_in(nc, "w0bc", [128, 512])
        a0_d = dram_in(nc, "a0T", [128, 4])
        kk_d = dram_in(nc, "kkT", [128, 2])
        ka_d = dram_in(nc, "kaT", [128, 2])
        rk_d = dram_in(nc, "rkT", [128, 2])
        tri_d = dram_in(nc, "tri", [128, 512])
        bones_d = dram_in(nc, "bones", [128, 128])
        scr = dram_out(nc, "scr", [2, 2, 5, 128, TB], BF16)
        gT = dram_out(nc, "gT", [256, TB])
        bonT = dram_out(nc, "bonT", [256, TB])
        gC_o = dram_out(nc, "gC", [4, 128, NCHUNK])

        def ld(shape, src_ap, dt=F32, q="sp", name=None):
            t = P.sb(shape, dt, name)
            P.load(t[:], src_ap, q=q)
            return t
        cw = ld([128, 24], cw_d)
        wup = ld([64, 512], wup_d, BF16, "pool")
        aup = P.sb([128, 512], BF16, "aup")
        P.load(aup[64:128, :], aup_d, q="pool")
        gup = ld([128, 256], gup_d, BF16, "pool")
        w0bc = ld([128, 512], w0bc_d)
        a0 = ld([128, 4], a0_d)
        kkc = ld([128, 2], kk_d)
        kac = ld([128, 2], ka_d)
        rkc = ld([128, 2], rk_d)
        tri = ld([128, 512], tri_d)
        bones = ld([128, 128], bones_d)
        e12 = P.sb([128, 1], F32)
        P.memset(e12[:], 1e-12)
        gCt = [P.sb([128, NCHUNK], F32, f"gC{i}") for i in range(4)]

        xin = [P.sb([128, 8, 514], F32, f"xin{i}") for i in range(2)]
        co = P.sb([128, 8, 512], F32, "convo")
        twb = P.sb([64, 512], BF16, "twb")
        adb = P.sb([128, 512], BF16, "adb")
        sgb = P.sb([128, 512], BF16, "sgb")
        names = ["kkr", "sq", "rinv", "kap", "rkk", "bon", "gsb", "a", "kt", "bb", "lw", "EG", "EnG", "EGx"]
        S = {nm: P.sb([128, 512], F32, "pp_" + nm) for nm in names}
        ob = [P.sb([128, 5, 512], BF16, f"ob{i}") for i in range(2)]
        Bk = [P.ps([128, 512], F32, f"bank{i}") for i in range(6)]
        pv = pin.rearrange("(i p) t -> p i t", p=128)
        oi = 0
        for bi, (t0, n, s0, s1) in enumerate(rwkv_blocks()):
            x = xin[bi % 2]
            hl = 1 if t0 > s0 else 0
            hr = 1 if t0 + n < s1 else 0
            if not hl:
                P.memset(x[:, :, 0:1], 0.0, eng="pool")
            if not hr:
                P.memset(x[:, :, n + 1:n + 2], 0.0, eng="pool")
            P.load(x[:, :, 1 - hl:n + 1 + hr], pv[:, :, t0 - hl:t0 + n + hr], q="sp")
            for i in range(8):
                eng = "dve"
                P.ts(co[:, i, :n], x[:, i, 1:n + 1], cw[:, 3 * i + 1:3 * i + 2], None, ALU.mult, eng=eng)
                P.stt(co[:, i, :n], x[:, i, 0:n], cw[:, 3 * i:3 * i + 1], co[:, i, :n], ALU.mult, ALU.add, eng=eng)
                P.stt(co[:, i, :n], x[:, i, 2:n + 2], cw[:, 3 * i + 2:3 * i + 3], co[:, i, :n], ALU.mult, ALU.add, eng=eng)
            P.act(twb[:, :n], co[0:64, 6, :n], AF.Tanh)
            P.cp(adb[64:128, :n], co[64:128, 6, :n], eng="act")
            P.act(sgb[:, :n], co[:, 7, :n], AF.Sigmoid)
            for hp in range(2):
                r, k, v = co[:, hp, :n], co[:, 2 + hp, :n], co[:, 4 + hp, :n]
                P.ts(S["kkr"][:, :n], k, kkc[:, hp:hp + 1], None, ALU.mult)
                P.act(S["sq"][:, :n], S["kkr"][:, :n], AF.Square)
                P.mm(Bk[0][:, :n], bones[:], S["sq"][:, :n])
                P.act(S["rinv"][:, :n], Bk[0][:, :n], AF.Sqrt, bias=e12[:, 0:1], scale=1.0)
                P.recip(S["rinv"][:, :n], S["rinv"][:, :n])
                P.tt(S["kap"][:, :n], S["kkr"][:, :n], S["rinv"][:, :n], ALU.mult)
                P.tt(S["rkk"][:, :n], r, k, ALU.mult)
                P.ts(S["rkk"][:, :n], S["rkk"][:, :n], rkc[:, hp:hp + 1], None, ALU.mult)
                P.mm(Bk[0][:, :n], bones[:], S["rkk"][:, :n])
                P.tt(S["bon"][:, :n], Bk[0][:, :n], v, ALU.mult)
                P.store(bonT[hp * 128:(hp + 1) * 128, t0:t0 + n], S["bon"][:, :n], q="act")
                P.mm(Bk[1][:, :n], gup[:, hp * 128:(hp + 1) * 128], sgb[:, :n])
                P.cp(S["gsb"][:, :n], Bk[1][:, :n], eng="act")
                P.store(gT[hp * 128:(hp + 1) * 128, t0:t0 + n], S["gsb"][:, :n], q="act")
                for z in range(2):
                    o = ob[oi % 2]
                    oi += 1
                    zc = z * 256 + hp * 128
                    P.mm(Bk[2][:, :n], aup[64:128, zc:zc + 128], adb[64:128, :n])
                    P.act(S["a"][:, :n], Bk[2][:, :n], AF.Sigmoid, bias=a0[:, z * 2 + hp:z * 2 + hp + 1], scale=1.0)
                    P.ts(S["kt"][:, :n], S["a"][:, :n], 1.0, kac[:, hp:hp + 1], ALU.subtract, ALU.mult)
                    P.stt(S["kt"][:, :n], S["kt"][:, :n], 1.0, k, ALU.add, ALU.mult)
                    P.tt(S["bb"][:, :n], S["a"][:, :n], S["kap"][:, :n], ALU.mult)
                    for tb in range(n // 128):
                        cs = slice(tb * 128, (tb + 1) * 128)
                        P.mm(Bk[3][:, :128], twb[0:64, cs], wup[0:64, zc:zc + 128])
                        P.tt(S["lw"][:, :128], Bk[3][:, :128], w0bc[:, zc:zc + 128], ALU.add)
                        P.act(S["lw"][:, :128], S["lw"][:, :128], AF.Sigmoid)
                        P.mm(Bk[4][:, cs], S["lw"][:, :128], tri[:, (2 * z) * 128:(2 * z + 1) * 128])
                        P.mm(Bk[5][:, cs], S["lw"][:, :128], tri[:, (2 * z + 1) * 128:(2 * z + 2) * 128])
                    P.act(S["EG"][:, :n], Bk[4][:, :n], AF.Exp, scale=-DECAY_SCALE)
                    P.act(S["EnG"][:, :n], Bk[4][:, :n], AF.Exp, scale=DECAY_SCALE)
                    P.act(S["EGx"][:, :n], Bk[5][:, :n], AF.Exp, scale=-DECAY_SCALE)
                    P.tt(o[:, 0, :n], r, S["EG"][:, :n], ALU.mult)
                    P.tt(o[:, 1, :n], S["kap"][:, :n], S["EGx"][:, :n], ALU.mult)
                    P.tt(o[:, 2, :n], S["bb"][:, :n], S["EnG"][:, :n], ALU.mult)
                    P.tt(o[:, 3, :n], S["kt"][:, :n], S["EnG"][:, :n], ALU.mult)
                    P.cp(o[:, 4, :n], v, eng="act")
                    c0 = t0 // CH
                    ncb = n // CH
                    col = CH - 1 if z == 0 else 0
                    P.cp(gCt[z * 2 + hp][:, c0:c0 + ncb], S["EG"][:, col:n:CH], eng="act")
                    P.store(scr[z, hp, :, :, t0:t0 + n].rearrange("a p t -> p a t"), o[:, :, :n], q="sp",
                            w=[("scr", z, hp, bi)])
        for i in range(4):
            P.store(gC_o[i], gCt[i][:], q="act", w=[("gC", i)])
        if phase_s:
            emit_rwkv_scan(nc, P, scr, gC_o, Bk)
    return build


NLVL = 14


def build_stage_s5():
    def build(nc, P):
        uT = dram_in(nc, "uT", [2, 256, TB])
        lr_d = dram_in(nc, "lrT", [128, 16])
        li_d = dram_in(nc, "liT", [128, 16])
        ldt_d = dram_in(nc, "ldtT", [128, 16])
        bre_d = dram_in(nc, "breT", [16, 32, 128])
        bim_d = dram_in(nc, "bimT", [16, 32, 128])
        cre_d = dram_in(nc, "creT", [16, 128, 32])
        cim_d = dram_in(nc, "cimT", [16, 128, 32])
        dd_d = dram_in(nc, "dT", [32, 8])
        yT = dram_out(nc, "yT", [2, 256, TB])

        def ld(shape, src_ap, name=None):
            t = P.sb(shape, F32, name)
            P.load(t[:], src_ap)
            return t
        lr, li, ldt = ld([128, 16], lr_d), ld([128, 16], li_d), ld([128, 16], ldt_d)
        dd = ld([32, 8], dd_d)
        bre = ld([32, 16, 128], bre_d.rearrange("t c p -> c t p"))
        bim = ld([32, 16, 128], bim_d.rearrange("t c p -> c t p"))
        cre = ld([128, 16, 32], cre_d.rearrange("t p c -> p t c"))
        cim = ld([128, 16, 32], cim_d.rearrange("t p c -> p t c"))
        hpi = P.sb([128, 1], F32)
        P.memset(hpi[:], math.pi / 2)
        T_ = lambda nm: P.sb([128, 16], F32, "d_" + nm)
        dt, xr, th, c0, s0, t1, t2, t3 = (T_(n) for n in ("dt", "xr", "th", "c0", "s0", "t1", "t2", "t3"))
        P.act(dt[:], ldt[:], AF.Exp)
        P.tt(xr[:], lr[:], dt[:], ALU.mult)
        P.tt(th[:], li[:], dt[:], ALU.mult)
        P.act(c0[:], th[:], AF.Sin, bias=hpi[:, 0:1], scale=1.0 / 8)
        P.act(s0[:], th[:], AF.Sin, scale=1.0 / 8)

        def sq_phasor(c, s):
            P.tt(t1[:], c[:], c[:], ALU.mult)
            P.tt(t2[:], s[:], s[:], ALU.mult)
            P.tt(t3[:], c[:], s[:], ALU.mult)
            P.tt(c[:], t1[:], t2[:], ALU.subtract)
            P.ts(s[:], t3[:], 2.0, None, ALU.mult)
            P.tt(t1[:], c[:], c[:], ALU.mult)
            P.tt(t2[:], s[:], s[:], ALU.mult)
            P.tt(t1[:], t1[:], t2[:], ALU.add)
            P.act(t1[:], t1[:], AF.Sqrt)
            P.recip(t1[:], t1[:])
            P.tt(c[:], c[:], t1[:], ALU.mult)
            P.tt(s[:], s[:], t1[:], ALU.mult)
        for _ in range(3):
            sq_phasor(c0, s0)
        ar = P.sb([128, NLVL, 16], F32, "ar")
        ai = P.sb([128, NLVL, 16], F32, "ai")
        nai = P.sb([128, NLVL, 16], F32, "nai")
        mag = T_("mag")
        for l in range(NLVL):
            P.act(mag[:], xr[:], AF.Exp, scale=float(2 ** l))
            P.tt(ar[:, l, :], c0[:], mag[:], ALU.mult)
            P.tt(ai[:, l, :], s0[:], mag[:], ALU.mult)
            P.ts(nai[:, l, :], ai[:, l, :], -1.0, None, ALU.mult)
            if l < NLVL - 1:
                sq_phasor(c0, s0)
        cr, ci, den, nr = T_("cr"), T_("ci"), T_("den"), T_("nr")
        P.ts(nr[:], ar[:, 0, :], -1.0, None, ALU.add)
        P.tt(t1[:], lr[:], lr[:], ALU.mult)
        P.tt(t2[:], li[:], li[:], ALU.mult)
        P.tt(den[:], t1[:], t2[:], ALU.add)
        P.recip(den[:], den[:])
        P.tt(t1[:], nr[:], lr[:], ALU.mult)
        P.tt(t2[:], ai[:, 0, :], li[:], ALU.mult)
        P.tt(t1[:], t1[:], t2[:], ALU.add)
        P.tt(cr[:], t1[:], den[:], ALU.mult)
        P.tt(t1[:], ai[:, 0, :], lr[:], ALU.mult)
        P.tt(t2[:], nr[:], li[:], ALU.mult)
        P.tt(t1[:], t1[:], t2[:], ALU.subtract)
        P.tt(ci[:], t1[:], den[:], ALU.mult)
        nci = T_("nci")
        P.ts(nci[:], ci[:], -1.0, None, ALU.mult)

        H = [[P.sb([128, TB], F32, f"h{a}{b}") for b in "ri"] for a in range(2)]
        u = P.sb([32, TB], F32, "u_sb")
        Bk = [P.ps([128, 512], F32, f"bank{i}") for i in range(6)]
        tmp = P.sb([128, 512], F32, "tmpx")
        ysb = [P.sb([32, 512], F32, f"ysb{i}") for i in range(2)]
        yi = P.sb([32, 512], F32, "yi_sb")
        oi = 0
        for z in range(2):
            for gp in range(8):
                ti = z * 8 + gp
                P.load(u[:], uT[z, gp * 32:(gp + 1) * 32, :], q="act")
                cur = 0
                hr, hi_ = H[0]
                for t0 in range(0, TB, 512):
                    n = min(512, TB - t0)
                    pr, pi_ = Bk[(t0 // 512) % 2 * 2], Bk[(t0 // 512) % 2 * 2 + 1]
                    P.mm(pr[:, :n], bre[:, ti, :], u[:, t0:t0 + n])
                    P.mm(pi_[:, :n], bim[:, ti, :], u[:, t0:t0 + n])
                    P.ts(tmp[:, :n], pr[:, :n], cr[:, ti:ti + 1], None, ALU.mult)
                    P.stt(hr[:, t0:t0 + n], pi_[:, :n], nci[:, ti:ti + 1], tmp[:, :n], ALU.mult, ALU.add)
                    P.ts(tmp[:, :n], pr[:, :n], ci[:, ti:ti + 1], None, ALU.mult)
                    P.stt(hi_[:, t0:t0 + n], pi_[:, :n], cr[:, ti:ti + 1], tmp[:, :n], ALU.mult, ALU.add)
                for l in range(NLVL):
                    s = 2 ** l
                    (sr, si), (dr, di) = H[cur], H[1 - cur]
                    a_r, a_i, na_i = ar[:, l, ti:ti + 1], ai[:, l, ti:ti + 1], nai[:, l, ti:ti + 1]
                    P.cp(dr[:, :s], sr[:, :s], eng="pool")
                    P.cp(di[:, :s], si[:, :s], eng="pool")
                    P.stt(dr[:, s:], sr[:, :TB - s], a_r, sr[:, s:], ALU.mult, ALU.add)
                    P.stt(dr[:, s:], si[:, :TB - s], na_i, dr[:, s:], ALU.mult, ALU.add)
                    P.stt(di[:, s:], sr[:, :TB - s], a_i, si[:, s:], ALU.mult, ALU.add)
                    P.stt(di[:, s:], si[:, :TB - s], a_r, di[:, s:], ALU.mult, ALU.add)
                    cur = 1 - cur
                hr, hi_ = H[cur]
                for t0 in range(0, TB, 512):
                    n = min(512, TB - t0)
                    pr, pi_ = Bk[4], Bk[5]
                    ys = ysb[oi % 2]
                    oi += 1
                    P.mm(pr[:32, :n], cre[:, ti, :], hr[:, t0:t0 + n])
                    P.mm(pi_[:32, :n], cim[:, ti, :], hi_[:, t0:t0 + n])
                    P.cp(yi[:, :n], pi_[:32, :n], eng="act")
                    P.tt(ys[:, :n], pr[:32, :n], yi[:, :n], ALU.subtract)
                    if z == 0:
                        P.stt(ys[:, :n], u[:, t0:t0 + n], dd[:, gp:gp + 1], ys[:, :n], ALU.mult, ALU.add)
                    P.store(yT[z, gp * 32:(gp + 1) * 32, t0:t0 + n], ys[:, :n], q="sp")
    return build


GN_EPS = 64e-5


def build_stage_m():
    T = TA

    def build(nc, P):
        yf = dram_in(nc, "yf", [512, T])
        yb = dram_in(nc, "yb", [512, T])
        bon = dram_in(nc, "bon", [512, T])
        gg = dram_in(nc, "gg", [512, T])
        sf = dram_in(nc, "sf", [512, T])
        sb_ = dram_in(nc, "sb", [512, T])
        lnw_d = dram_in(nc, "lnwT", [128, 4])
        lnb_d = dram_in(nc, "lnbT", [128, 4])
        glub_d = dram_in(nc, "glubT", [128, 4])
        gluw_d = dram_in(nc, "gluW", [4, 128, 128])
        bones_d = dram_in(nc, "bones", [128, 128])
        catT = dram_out(nc, "catT", [D, T])

        def ld(shape, src_ap):
            t = P.sb(shape, F32)
            P.load(t[:], src_ap)
            return t
        lnw, lnb, glub = ld([128, 4], lnw_d), ld([128, 4], lnb_d), ld([128, 4], glub_d)
        gluw = ld([128, 4, 128], gluw_d.rearrange("i p e -> p i e"))
        bones = ld([128, 128], bones_d)
        epsg = P.sb([128, 1], F32)
        P.memset(epsg[:], GN_EPS)
        Bk = [P.ps([128, 512], F32, f"bank{i}") for i in range(4)]
        nb = 2
        X = {nm: [P.sb([128, 512], F32, f"m_{nm}{i}") for i in range(nb)] for nm in
             ("a", "b", "c", "d", "y", "cen", "sq", "rs", "o")}
        it = 0
        for (t0, n, col) in token_blocks(4096, 128):
            for i in range(4):
                k = it % nb
                it += 1
                rows = slice(i * 128, (i + 1) * 128)
                a, b_, c_, d_, y, cen, sq, rs, o = (X[nm][k] for nm in ("a", "b", "c", "d", "y", "cen", "sq", "rs", "o"))
                P.load(a[:, :n], yf[rows, t0:t0 + n], q="sp")
                P.load(b_[:, :n], yb[rows, t0:t0 + n], q="act")
                P.load(c_[:, :n], bon[rows, t0:t0 + n], q="sp")
                P.load(d_[:, :n], gg[rows, t0:t0 + n], q="act")
                P.tt(y[:, :n], a[:, :n], b_[:, :n], ALU.add)
                P.mm(Bk[0][:, :n], bones[:], y[:, :n])
                P.stt(cen[:, :n], Bk[0][:, :n], -1.0 / 64, y[:, :n], ALU.mult, ALU.add)
                P.act(sq[:, :n], cen[:, :n], AF.Square)
                P.mm(Bk[1][:, :n], bones[:], sq[:, :n])
                P.act(rs[:, :n], Bk[1][:, :n], AF.Sqrt, bias=epsg[:, 0:1], scale=1.0 / 64)
                P.recip(rs[:, :n], rs[:, :n])
                P.tt(cen[:, :n], cen[:, :n], rs[:, :n], ALU.mult)
                P.ts(cen[:, :n], cen[:, :n], lnw[:, i:i + 1], lnb[:, i:i + 1], ALU.mult, ALU.add)
                P.tt(cen[:, :n], cen[:, :n], c_[:, :n], ALU.add)
                P.tt(o[:, :n], cen[:, :n], d_[:, :n], ALU.mult)
                P.store(catT[rows, t0:t0 + n], o[:, :n], q="sp")
            for i in range(4):
                k = it % nb
                it += 1
                rows = slice(i * 128, (i + 1) * 128)
                a, b_, y, cen, sq, rs, o = (X[nm][k] for nm in ("a", "b", "y", "cen", "sq", "rs", "o"))
                P.load(a[:, :n], sf[rows, t0:t0 + n], q="sp")
                P.load(b_[:, :n], sb_[rows, t0:t0 + n], q="act")
                P.tt(y[:, :n], a[:, :n], b_[:, :n], ALU.add)
                P.act(sq[:, :n], y[:, :n], AF.Square)
                P.ts(sq[:, :n], sq[:, :n], 0.044715, 1.0, ALU.mult, ALU.add)
                P.tt(sq[:, :n], sq[:, :n], y[:, :n], ALU.mult)
                P.act(rs[:, :n], sq[:, :n], AF.Tanh, scale=0.7978845608028654)
                P.ts(rs[:, :n], rs[:, :n], 1.0, 0.5, ALU.add, ALU.mult)
                P.tt(cen[:, :n], rs[:, :n], y[:, :n], ALU.mult)
                P.mm(Bk[2][:, :n], gluw[:, i, :], cen[:, :n])
                P.act(rs[:, :n], Bk[2][:, :n], AF.Sigmoid, bias=glub[:, i:i + 1], scale=1.0)
                P.tt(o[:, :n], cen[:, :n], rs[:, :n], ALU.mult)
                P.store(catT[512 + i * 128:512 + (i + 1) * 128, t0:t0 + n], o[:, :n], q="sp")
    return build


def _stage_a_inputs(inp, h_lat, h_ctx, layer, g_name, w):
    maps = []
    for c in range(8):
        b, half = c // 2, c % 2
        xT = np.concatenate([h_lat[b, half * 4096:(half + 1) * 4096].T,
                             h_ctx[b, half * 128:(half + 1) * 128].T], 1)
        maps.append(dict(xT=np.ascontiguousarray(xT),
                         cvec=np.ascontiguousarray(np.stack([inp['c'][b], inp['c_ctx']], 1)),
                         modw=inp['mod_w'][layer],
                         modbT=np.ascontiguousarray(inp['mod_b'][layer].reshape(48, 128).T),
                         g1T=np.ascontiguousarray(inp[g_name][layer].reshape(8, 128).T), w_in=w))
    return maps


def _rope_tables():
    t = np.arange(8192)
    row = (t // 64).astype(np.float32)
    colp = (t % 64).astype(np.float32)
    inv = (np.float32(10000.0) ** (-np.arange(0, 16, 2, dtype=np.float32) / np.float32(16))).astype(np.float32)
    ang = np.concatenate([row[:, None] * inv, colp[:, None] * inv], -1).astype(np.float32)
    cos = np.ones((16, NKEY), np.float32)
    sin = np.zeros((16, NKEY), np.float32)
    cos[:, :8192] = np.cos(ang).T
    sin[:, :8192] = np.sin(ang).T
    cT = np.zeros((128, NKEY), np.float32)
    sT = np.zeros((128, NKEY), np.float32)
    for lo in (64, 96):
        cT[lo:lo + 16] = cos
        sT[lo:lo + 16] = sin
    return cT, sT


def _stage_d_inputs(inp, h_lat, h_ctx, cores=range(8)):
    layer = 1
    w = inp['mla_w_in'][0]
    qup = inp['mla_q_up'][0]
    kvup = inp['mla_kv_up'][0]
    wka = np.zeros((D, QR), np.float32)
    wkb = np.zeros((D, QR), np.float32)
    for lo in (64, 96):
        wka[:, lo:lo + 16] = w[:, 384:400]
        wkb[:, lo:lo + 16] = w[:, 400:416]
    wqa = np.zeros((256, NH, QR), np.float32)
    wqb = np.zeros((256, NH, QR), np.float32)
    q3 = qup.reshape(256, NH, 96)
    wqa[:, :, 0:64] = q3[:, :, 0:64]
    for lo in (64, 96):
        wqa[:, :, lo:lo + 16] = q3[:, :, 64:80]
        wqb[:, :, lo:lo + 16] = q3[:, :, 80:96]
    kv3 = kvup.reshape(128, NH, 128)
    kupk = np.ascontiguousarray(kv3[:, :, :64].reshape(128, NH * 64))
    kupv = np.ascontiguousarray(kv3[:, :, 64:].reshape(128, NH * 64))
    cT, sT = _rope_tables()
    maps = []
    for c in cores:
        b, qh = c // 2, c % 2
        hT = np.ascontiguousarray(np.concatenate([h_lat[b].T, h_ctx[b].T], 1))
        maps.append(dict(hT=hT, hqT=np.ascontiguousarray(hT[:, qh * NQ:(qh + 1) * NQ]),
                         cosq=np.ascontiguousarray(cT[:, qh * NQ:(qh + 1) * NQ]),
                         sinq=np.ascontiguousarray(sT[:, qh * NQ:(qh + 1) * NQ]), cvec=np.ascontiguousarray(np.stack([inp['c'][b], inp['c_ctx']], 1)),
                         modw=inp['mod_w'][layer],
                         modbT=np.ascontiguousarray(inp['mod_b'][layer].reshape(48, 128).T),
                         g1T=np.ascontiguousarray(inp['norm1_g'][layer].reshape(8, 128).T),
                         win=np.ascontiguousarray(w[:, :384]), wka=wka, wkb=wkb,
                         wqa=wqa.reshape(256, NH * QR), wqb=wqb.reshape(256, NH * QR),
                         kupk=kupk, kupv=kupv,
                         gq=np.ascontiguousarray(inp['mla_q_norm'][0].reshape(2, 128).T),
                         gkv=np.ascontiguousarray(inp['mla_kv_norm'][0][:, None]),
                         cosT=cT, sinT=sT))
    return maps


def _stage_b_consts():
    t = np.arange(128)
    same = (t[:, None] // CH) == (t[None, :] // CH)
    tri = np.concatenate([(same & (t[:, None] <= t[None, :])), (same & (t[:, None] < t[None, :])),
                          (same & (t[:, None] >= t[None, :])), (same & (t[:, None] > t[None, :]))], 1).astype(np.float32)
    bones = same.astype(np.float32)
    return tri, bones


def _scan_consts():
    i = np.arange(64)
    up_s = (i[:, None] < i[None, :]).astype(np.float32)
    up_i = (i[:, None] <= i[None, :]).astype(np.float32)
    lo_s = (i[:, None] > i[None, :]).astype(np.float32)
    lo_i = (i[:, None] >= i[None, :]).astype(np.float32)
    rep = lambda f, bw: np.concatenate([f] * 4 + [bw] * 4, 1)
    masks = np.concatenate([rep(up_s, lo_s), rep(up_i, lo_i), rep(lo_s, up_s)], 1)
    return np.eye(64, dtype=np.float32), np.ascontiguousarray(masks)


def _stage_b_inputs(inp, pl, pc, cores=range(8)):
    tri, bones = _stage_b_consts()
    conv = inp['rk_conv'][0]
    maps = []
    for c in cores:
        b, j = c // 2, c % 2
        ch = np.arange(256) + 256 * j
        sel = np.concatenate([ch, 512 + ch, 1024 + ch, np.arange(1536, 1792)])
        pb = np.concatenate([pl[b][:, sel], pc[b][:, sel]], 0)
        cw = np.ascontiguousarray(conv[:, sel].T.reshape(8, 128, 3).transpose(1, 0, 2).reshape(128, 24))
        wup = np.ascontiguousarray(inp['rk_w_up'][0][:, :, ch].transpose(1, 0, 2).reshape(64, 512))
        aup = np.ascontiguousarray(inp['rk_a_up'][0][:, :, ch].transpose(1, 0, 2).reshape(64, 512))
        gup = np.ascontiguousarray(inp['rk_g_up'][0][:, ch])
        w0bc = np.ascontiguousarray(np.tile(inp['rk_w0'][0][:, ch].reshape(1, 512), (128, 1)))
        a0T = np.ascontiguousarray(inp['rk_a0'][0][:, ch].reshape(2, 2, 128).transpose(2, 0, 1).reshape(128, 4))
        v2 = lambda a: np.ascontiguousarray(a[ch].reshape(2, 128).T)
        maps.append(dict(pin=np.ascontiguousarray(pb.T), cw=cw, wup=wup, aup=aup, gup=gup, w0bc=w0bc, a0T=a0T,
                         kkT=v2(inp['rk_k_k'][0]), kaT=v2(inp['rk_k_a'][0]), rkT=v2(inp['rk_r_k'][0].reshape(512)),
                         tri=tri, bones=bones, ident64=_scan_consts()[0], masks=_scan_consts()[1]))
    return maps


def _scan_order(a_lat, a_ctx, z):
    if z == 0:
        return np.concatenate([a_ctx, a_lat], 0).T
    return np.concatenate([a_ctx[::-1], a_lat[::-1]], 0).T


def _stage_s5_inputs(inp, pl, pc, cores=range(8)):
    maps = []
    lam_re, lam_im, log_dt = inp['s5_lam_re'][0], inp['s5_lam_im'][0], inp['s5_log_dt'][0]
    bre, bim, cre, cim = inp['s5_b_re'][0], inp['s5_b_im'][0], inp['s5_c_re'][0], inp['s5_c_im'][0]
    for c in cores:
        b, j = c // 2, c % 2
        ch = 1792 + 256 * j + np.arange(256)
        uT = np.stack([_scan_order(pl[b][:, ch], pc[b][:, ch], z) for z in range(2)], 0)
        lrT = np.zeros((128, 16), np.float32)
        liT = np.zeros((128, 16), np.float32)
        ldtT = np.zeros((128, 16), np.float32)
        breT = np.zeros((16, 32, 128), np.float32)
        bimT = np.zeros((16, 32, 128), np.float32)
        creT = np.zeros((16, 128, 32), np.float32)
        cimT = np.zeros((16, 128, 32), np.float32)
        for z in range(2):
            for gp in range(8):
                ti = z * 8 + gp
                for g2 in range(2):
                    g = 16 * j + 2 * gp + g2
                    ps_ = slice(g2 * 64, (g2 + 1) * 64)
                    cs_ = slice(g2 * 16, (g2 + 1) * 16)
                    lrT[ps_, ti] = lam_re[z, g]
                    liT[ps_, ti] = lam_im[z, g]
                    ldtT[ps_, ti] = log_dt[z, g]
                    breT[ti, cs_, ps_] = bre[z, g].T
                    bimT[ti, cs_, ps_] = bim[z, g].T
                    creT[ti, ps_, cs_] = cre[z, g].T
                    cimT[ti, ps_, cs_] = cim[z, g].T
        dT = np.ascontiguousarray(inp['s5_d'][0][256 * j:256 * j + 256].reshape(8, 32).T)
        maps.append(dict(uT=np.ascontiguousarray(uT), lrT=lrT, liT=liT, ldtT=ldtT, breT=breT, bimT=bimT,
                         creT=creT, cimT=cimT, dT=dT))
    return maps


def _tail_inputs(inp, layer, catT_list, hT_list, w_out):
    ident = np.eye(128, dtype=np.float32)
    sel = np.zeros((16, 16 * 128), np.float32)
    for e in range(16):
        sel[e, e * 128:(e + 1) * 128] = 1.0
    exg = np.concatenate([inp['ex_gate'][layer], inp['sh_gate'][layer][None]], 0)
    exu = np.concatenate([inp['ex_up'][layer], inp['sh_up'][layer][None]], 0)
    exd = np.concatenate([inp['ex_down'][layer], inp['sh_down'][layer][None]], 0)
    rbb = np.ascontiguousarray(np.tile(inp['router_b'][None], (128, 1)))
    maps = []
    for c in range(8):
        b = c // 2
        maps.append(dict(catT=np.ascontiguousarray(catT_list[c]), hT=np.ascontiguousarray(hT_list[c]),
                         cvec=np.ascontiguousarray(np.stack([inp['c'][b], inp['c_ctx']], 1)),
                         modw=inp['mod_w'][layer],
                         modbT=np.ascontiguousarray(inp['mod_b'][layer].reshape(48, 128).T),
                         g2T=np.ascontiguousarray(inp['norm2_g'][layer].reshape(8, 128).T),
                         gfT=np.ascontiguousarray(inp['final_g'].reshape(8, 128).T),
                         w_out=w_out, rw=inp['router_w'], rbb=rbb, exg=exg, exu=exu, exd=exd, ident=ident, sel=sel))
    return maps


def _tok_cols(half):
    return np.concatenate([np.arange(half * 4096, (half + 1) * 4096), 8192 + np.arange(half * 128, (half + 1) * 128)])


def kernel(**inp):
    inp = {k: np.asarray(v) for k, v in inp.items()}
    x, ctx = inp['x'], inp['ctx']
    resA = run_prog(build_stage_a, _stage_a_inputs(inp, x, ctx, 0, 'norm1_g', inp['hy_w_in'][0]))
    pl = np.empty((4, 8192, HYB_IN), np.float32)
    pc = np.empty((4, 256, HYB_IN), np.float32)
    for c in range(8):
        b, half = c // 2, c % 2
        pT = resA[c]["pT"]
        pl[b, half * 4096:(half + 1) * 4096] = pT[:, :4096].T
        pc[b, half * 128:(half + 1) * 128] = pT[:, 4096:].T
    del resA
    resB = run_prog(build_stage_b(True), _stage_b_inputs(inp, pl, pc))
    resS = run_prog(build_stage_s5(), _stage_s5_inputs(inp, pl, pc))
    yf = np.empty((4, 512, TB), np.float32)
    yb = np.empty((4, 512, TB), np.float32)
    bon = np.empty((4, 512, TB), np.float32)
    gg = np.empty((4, 512, TB), np.float32)
    sf = np.empty((4, 512, TB), np.float32)
    sbk = np.empty((4, 512, TB), np.float32)
    for c in range(8):
        b, j = c // 2, c % 2
        rows = slice(256 * j, 256 * j + 256)
        yT = resB[c]["yT"]
        yf[b, rows] = yT[0].reshape(256, TB)
        yb[b, rows] = yT[1].reshape(256, TB)
        bon[b, rows] = resB[c]["bonT"]
        gg[b, rows] = resB[c]["gT"]
        s = resS[c]["yT"]
        sf[b, rows] = np.concatenate([s[0][:, 256:], s[0][:, :256]], 1)
        sbk[b, rows] = np.concatenate([s[1][:, 256:][:, ::-1], s[1][:, :256][:, ::-1]], 1)
    del resB, resS
    tri, bones = _stage_b_consts()
    v4 = lambda a: np.ascontiguousarray(a.reshape(4, 128).T)
    gluW = np.zeros((4, 128, 128), np.float32)
    for g in range(32):
        i, g8 = g // 8, g % 8
        gluW[i, g8 * 16:(g8 + 1) * 16, g8 * 16:(g8 + 1) * 16] = inp['s5_glu_w'][0][g]
    mapsM = []
    for c in range(8):
        b, half = c // 2, c % 2
        cols = _tok_cols(half)
        mapsM.append(dict(yf=np.ascontiguousarray(yf[b][:, cols]), yb=np.ascontiguousarray(yb[b][:, cols]),
                          bon=np.ascontiguousarray(bon[b][:, cols]), gg=np.ascontiguousarray(gg[b][:, cols]),
                          sf=np.ascontiguousarray(sf[b][:, cols]), sb=np.ascontiguousarray(sbk[b][:, cols]),
                          lnwT=v4(inp['rk_ln_w'][0]), lnbT=v4(inp['rk_ln_b'][0]), glubT=v4(inp['s5_glu_b'][0]),
                          gluW=gluW, bones=bones))
    resM = run_prog(build_stage_m(), mapsM)
    hT0 = []
    for c in range(8):
        b, half = c // 2, c % 2
        hT0.append(np.concatenate([x[b, half * 4096:(half + 1) * 4096].T, ctx[b, half * 128:(half + 1) * 128].T], 1))
    resT0 = run_prog(build_stage_tail(False, 4096, 128),
                     _tail_inputs(inp, 0, [r["catT"] for r in resM], hT0, inp['hy_w_out'][0]))
    del resM
    h_lat = np.empty((4, 8192, D), np.float32)
    h_ctx = np.empty((4, 256, D), np.float32)
    for c in range(8):
        b, half = c // 2, c % 2
        hT = resT0[c]["houtT"]
        h_lat[b, half * 4096:(half + 1) * 4096] = hT[:, :4096].T
        h_ctx[b, half * 128:(half + 1) * 128] = hT[:, 4096:].T
    del resT0
    resD = run_prog(build_stage_d(), _stage_d_inputs(inp, h_lat, h_ctx))
    hT1 = []
    for c in range(8):
        b, half = c // 2, c % 2
        hT1.append(h_lat[b, half * 4096:(half + 1) * 4096].T)
    resT1 = run_prog(build_stage_tail(True, 4096, 0),
                     _tail_inputs(inp, 1, [r["attT"] for r in resD], hT1, inp['mla_w_out'][0]))
    out = np.empty(x.shape, np.float32)
    for c in range(8):
        b, half = c // 2, c % 2
        out[b, half * 4096:(half + 1) * 4096] = resT1[c]["houtT"].T
    return out
```
